# Optimizing a Trainium2 kernel written in Bass

```python
import jax
import jax.numpy as jnp
from jax import lax
import numpy as np

D_MODEL = 1024
BATCH = 4
SEQ = 4096
DEPTH = 2

GRID_W = 64
CTX_LEN = 256
HEAD_DIM = 64
CONV_CH = D_MODEL // 2
CONV_WIDTH = 31
B_HEADS = (D_MODEL // 2) // HEAD_DIM
B_KV_HEADS = 2
Q_BLOCK = 128
ROPE_THETA = 10000.0
NA_HEADS = D_MODEL // HEAD_DIM
WIN_R = 8
WIN_C = 16
FFN_DIM = 2816
N_EXPERTS = 8
TOP_K = 2
EXPERT_DIM = 3584
N_EVEN = (DEPTH + 1) // 2
N_ODD = DEPTH // 2
ALPHA = (2 * DEPTH) ** 0.25
BETA = (8 * DEPTH) ** -0.25
LN_EPS = 1e-5
RMS_EPS = 1e-6
A_COLS = 2 * CONV_CH
Q_COLS = B_HEADS * HEAD_DIM
KV_COLS = B_KV_HEADS * HEAD_DIM
IN_AB = A_COLS + Q_COLS + 2 * KV_COLS
OUT_AB = CONV_CH + Q_COLS
NA_WIDTH = NA_HEADS * HEAD_DIM

kernel_name = 'hybrid_conv_gqa_natten_moe_diffusion_block'


def layer_norm(x, g, b):
    xf = x.astype(jnp.float32)
    xc = xf - jnp.mean(xf, axis=-1, keepdims=True)
    var = jnp.mean(xc * xc, axis=-1, keepdims=True)
    return (xc * lax.rsqrt(var + LN_EPS) * g + b).astype(x.dtype)


def rms_norm(x, g):
    xf = x.astype(jnp.float32)
    y = xf * lax.rsqrt(jnp.mean(xf * xf, axis=-1, keepdims=True) + RMS_EPS)
    return (y * g).astype(x.dtype)


def rope_axis(x, pos):
    d = x.shape[-1]
    half = d // 2
    inv = ROPE_THETA ** (-jnp.arange(half, dtype=jnp.float32) * 2.0 / d)
    ang = pos.astype(jnp.float32)[:, None] * inv[None, :]
    cos = jnp.cos(ang)[:, None, :]
    sin = jnp.sin(ang)[:, None, :]
    xf = x.astype(jnp.float32)
    x1, x2 = xf[..., :half], xf[..., half:]
    return jnp.concatenate([x1 * cos - x2 * sin, x2 * cos + x1 * sin], axis=-1).astype(x.dtype)


def rope_2d(x, rows, cols):
    h = x.shape[-1] // 2
    return jnp.concatenate([rope_axis(x[..., :h], rows), rope_axis(x[..., h:], cols)], axis=-1)


def conformer_conv(p, conv_w, conv_g, conv_b):
    u = p[..., :CONV_CH] * jax.nn.sigmoid(p[..., CONV_CH:])
    u = lax.conv_general_dilated(
        u, conv_w[:, None, :].astype(u.dtype), window_strides=(1,),
        padding=[(CONV_WIDTH // 2, CONV_WIDTH // 2)],
        dimension_numbers=('NWC', 'WIO', 'NWC'), feature_group_count=CONV_CH)
    return jax.nn.silu(layer_norm(u, conv_g, conv_b))


def gqa_attend(qb, keys, vals):
    bsz, nq = qb.shape[0], qb.shape[1]
    g = B_HEADS // B_KV_HEADS
    q5 = qb.reshape(bsz, nq, B_KV_HEADS, g, HEAD_DIM)
    s = jnp.einsum('bqkgd,bskd->bkgqs', q5, keys).astype(jnp.float32)
    p = jax.nn.softmax(s, axis=-1).astype(vals.dtype)
    o = jnp.einsum('bkgqs,bskd->bqkgd', p, vals)
    return o.reshape(bsz, nq, B_HEADS * HEAD_DIM)


def mixer_ab(h, hc, w_in, conv_w, conv_g, conv_b, q_g, k_g, w_out, rows, cols, update_ctx):
    bsz, t, _ = h.shape
    n_ctx = hc.shape[1]
    scale = HEAD_DIM ** -0.5
    p = h @ w_in
    a = conformer_conv(p[..., :A_COLS], conv_w, conv_g, conv_b)
    pb = p[..., A_COLS:]
    q = rms_norm(pb[..., :Q_COLS].reshape(bsz, t, B_HEADS, HEAD_DIM), q_g)
    k = rms_norm(pb[..., Q_COLS:Q_COLS + KV_COLS].reshape(bsz, t, B_KV_HEADS, HEAD_DIM), k_g)
    v = pb[..., Q_COLS + KV_COLS:].reshape(bsz, t, B_KV_HEADS, HEAD_DIM)
    q = rope_2d(q, rows, cols) * scale
    k = rope_2d(k, rows, cols)
    if update_ctx:
        pc = hc @ w_in
        pc_kv = pc[..., A_COLS + Q_COLS:]
    else:
        pc_kv = hc @ w_in[:, A_COLS + Q_COLS:]
    kc = rms_norm(pc_kv[..., :KV_COLS].reshape(bsz, n_ctx, B_KV_HEADS, HEAD_DIM), k_g)
    vc = pc_kv[..., KV_COLS:].reshape(bsz, n_ctx, B_KV_HEADS, HEAD_DIM)
    keys = jnp.concatenate([kc, k], axis=1)
    vals = jnp.concatenate([vc, v], axis=1)
    qs = q.reshape(bsz, t // Q_BLOCK, Q_BLOCK, B_HEADS, HEAD_DIM).transpose(1, 0, 2, 3, 4)
    o = lax.map(lambda qb: gqa_attend(qb, keys, vals), qs)
    o = o.transpose(1, 0, 2, 3).reshape(bsz, t, Q_COLS)
    y = jnp.concatenate([a, o], axis=-1) @ w_out
    if update_ctx:
        ac = conformer_conv(pc[..., :A_COLS], conv_g=conv_g, conv_w=conv_w, conv_b=conv_b)
        qc = rms_norm(pc[..., A_COLS:A_COLS + Q_COLS].reshape(bsz, n_ctx, B_HEADS, HEAD_DIM), q_g) * scale
        oc = gqa_attend(qc, kc, vc)
        yc = jnp.concatenate([ac, oc], axis=-1) @ w_out
    else:
        yc = None
    return y, yc


def mixer_na(h, hc, w_qkv, rpb, w_out, update_ctx):
    bsz, t, _ = h.shape
    n_ctx = hc.shape[1]
    n_rows = t // GRID_W
    kr = min(WIN_R, n_rows)
    scale = HEAD_DIM ** -0.5
    qkv = (h @ w_qkv).reshape(bsz, t, 3, NA_HEADS, HEAD_DIM)
    qg = (qkv[:, :, 0] * scale).reshape(bsz, n_rows, GRID_W, NA_HEADS, HEAD_DIM)
    kg = qkv[:, :, 1].reshape(bsz, n_rows, GRID_W, NA_HEADS, HEAD_DIM)
    vg = qkv[:, :, 2].reshape(bsz, n_rows, GRID_W, NA_HEADS, HEAD_DIM)
    if update_ctx:
        qkvc = (hc @ w_qkv).reshape(bsz, n_ctx, 3, NA_HEADS, HEAD_DIM)
        kc, vc = qkvc[:, :, 1], qkvc[:, :, 2]
    else:
        kvc = (hc @ w_qkv[:, NA_WIDTH:]).reshape(bsz, n_ctx, 2, NA_HEADS, HEAD_DIM)
        kc, vc = kvc[:, :, 0], kvc[:, :, 1]
    row_start = jnp.clip(jnp.arange(n_rows) - kr // 2, 0, n_rows - kr)
    col_start = jnp.clip(jnp.arange(GRID_W) - WIN_C // 2, 0, GRID_W - WIN_C)
    col_idx = col_start[:, None] + jnp.arange(WIN_C)[None, :]
    dc_idx = col_idx - jnp.arange(GRID_W)[:, None] + WIN_C - 1
    bias_c = rpb[:, :, dc_idx]

    def row_block(args):
        q_r, r = args
        r0 = row_start[r]
        k_rows = lax.dynamic_slice_in_dim(kg, r0, kr, axis=1)
        v_rows = lax.dynamic_slice_in_dim(vg, r0, kr, axis=1)
        k_win = k_rows[:, :, col_idx]
        v_win = v_rows[:, :, col_idx]
        dr_idx = r0 + jnp.arange(kr) - r + WIN_R - 1
        bias = bias_c[:, dr_idx].transpose(0, 2, 1, 3)
        s_win = jnp.einsum('bqhd,biqjhd->bhqij', q_r, k_win).astype(jnp.float32) + bias[None].astype(jnp.float32)
        s_ctx = jnp.einsum('bqhd,bshd->bhqs', q_r, kc).astype(jnp.float32)
        s = jnp.concatenate([s_win.reshape(bsz, NA_HEADS, GRID_W, kr * WIN_C), s_ctx], axis=-1)
        p = jax.nn.softmax(s, axis=-1).astype(vg.dtype)
        p_win = p[..., :kr * WIN_C].reshape(bsz, NA_HEADS, GRID_W, kr, WIN_C)
        p_ctx = p[..., kr * WIN_C:]
        return (jnp.einsum('bhqij,biqjhd->bqhd', p_win, v_win)
                + jnp.einsum('bhqs,bshd->bqhd', p_ctx, vc))

    o = lax.map(row_block, (qg.transpose(1, 0, 2, 3, 4), jnp.arange(n_rows)))
    o = o.transpose(1, 0, 2, 3, 4).reshape(bsz, t, NA_WIDTH)
    y = o @ w_out
    if update_ctx:
        qc = qkvc[:, :, 0] * scale
        sc = jnp.einsum('bqhd,bshd->bhqs', qc, kc).astype(jnp.float32)
        pc = jax.nn.softmax(sc, axis=-1).astype(vc.dtype)
        oc = jnp.einsum('bhqs,bshd->bqhd', pc, vc).reshape(bsz, n_ctx, NA_WIDTH)
        yc = oc @ w_out
    else:
        yc = None
    return y, yc


def swiglu(h, w_gate, w_up, w_down):
    return (jax.nn.silu(h @ w_gate) * (h @ w_up)) @ w_down


def moe_swiglu(h, w_router, w_gate, w_up, w_down):
    lead = h.shape[:-1]
    xt = h.reshape(-1, h.shape[-1])
    logits = (xt @ w_router).astype(jnp.float32)
    top_v, top_i = lax.top_k(logits, TOP_K)
    top_w = jax.nn.softmax(top_v, axis=-1)
    gates = jnp.einsum('nk,nke->ne', top_w, jax.nn.one_hot(top_i, N_EXPERTS, dtype=jnp.float32)).astype(h.dtype)
    y = jnp.zeros_like(xt)
    for e in range(N_EXPERTS):
        y = y + gates[:, e:e + 1] * swiglu(xt, w_gate[e], w_up[e], w_down[e])
    return y.reshape(*lead, -1)


def setup_inputs(seed: int = 0) -> dict:
    key = jax.random.key(seed)
    ks = jax.random.split(key, 25)

    def nrm(k, shape, s):
        return jax.random.normal(k, shape, jnp.float32) * s

    d = D_MODEL
    return {
        'x': nrm(ks[0], (BATCH, SEQ, d), 1.0),
        'c': nrm(ks[1], (BATCH, d), 1.0),
        'ctx': nrm(ks[2], (BATCH, CTX_LEN, d), 1.0),
        'c_ctx': nrm(ks[3], (d,), 1.0),
        'w_mod': nrm(ks[4], (DEPTH, d, 6 * d), 0.5 * d ** -0.5),
        'b_mod': nrm(ks[5], (DEPTH, 6 * d), 0.01),
        'ln_g': 1.0 + nrm(ks[6], (DEPTH, 2, d), 0.02),
        'ln_b': nrm(ks[7], (DEPTH, 2, d), 0.02),
        'ab_w_in': nrm(ks[8], (N_EVEN, d, IN_AB), d ** -0.5),
        'ab_conv_w': nrm(ks[9], (N_EVEN, CONV_WIDTH, CONV_CH), CONV_WIDTH ** -0.5),
        'ab_conv_g': 1.0 + nrm(ks[10], (N_EVEN, CONV_CH), 0.02),
        'ab_conv_b': nrm(ks[11], (N_EVEN, CONV_CH), 0.02),
        'ab_q_g': 1.0 + nrm(ks[12], (N_EVEN, HEAD_DIM), 0.02),
        'ab_k_g': 1.0 + nrm(ks[13], (N_EVEN, HEAD_DIM), 0.02),
        'ab_w_out': nrm(ks[14], (N_EVEN, OUT_AB, d), BETA * OUT_AB ** -0.5),
        'ffn_w_gate': nrm(ks[15], (N_EVEN, d, FFN_DIM), d ** -0.5),
        'ffn_w_up': nrm(ks[16], (N_EVEN, d, FFN_DIM), d ** -0.5),
        'ffn_w_down': nrm(ks[17], (N_EVEN, FFN_DIM, d), BETA * FFN_DIM ** -0.5),
        'na_w_qkv': nrm(ks[18], (N_ODD, d, 3 * NA_WIDTH), d ** -0.5),
        'na_rpb': nrm(ks[19], (N_ODD, NA_HEADS, 2 * WIN_R - 1, 2 * WIN_C - 1), 0.1),
        'na_w_out': nrm(ks[20], (N_ODD, NA_WIDTH, d), BETA * NA_WIDTH ** -0.5),
        'moe_w_router': nrm(ks[21], (N_ODD, d, N_EXPERTS), d ** -0.5),
        'moe_w_gate': nrm(ks[22], (N_ODD, N_EXPERTS, d, EXPERT_DIM), d ** -0.5),
        'moe_w_up': nrm(ks[23], (N_ODD, N_EXPERTS, d, EXPERT_DIM), d ** -0.5),
        'moe_w_down': nrm(ks[24], (N_ODD, N_EXPERTS, EXPERT_DIM, d), BETA * EXPERT_DIM ** -0.5),
    }


def reference(x, c, ctx, c_ctx, w_mod, b_mod, ln_g, ln_b, ab_w_in, ab_conv_w, ab_conv_g, ab_conv_b,
              ab_q_g, ab_k_g, ab_w_out, ffn_w_gate, ffn_w_up, ffn_w_down, na_w_qkv, na_rpb, na_w_out,
              moe_w_router, moe_w_gate, moe_w_up, moe_w_down):
    t = x.shape[1]
    pos = jnp.arange(t)
    rows = pos // GRID_W
    cols = pos % GRID_W
    for i in range(DEPTH):
        j = i // 2
        update_ctx = i < DEPTH - 1
        mod = (jax.nn.silu(c) @ w_mod[i] + b_mod[i])[:, None, :]
        sh_m, sc_m, g_m, sh_f, sc_f, g_f = jnp.split(mod, 6, axis=-1)
        modc = jax.nn.silu(c_ctx) @ w_mod[i] + b_mod[i]
        shc_m, scc_m, gc_m, shc_f, scc_f, gc_f = jnp.split(modc, 6, axis=-1)
        h = x * (1.0 + sc_m) + sh_m
        hc = ctx * (1.0 + scc_m) + shc_m
        if i % 2 == 0:
            y, yc = mixer_ab(h, hc, ab_w_in[j], ab_conv_w[j], ab_conv_g[j], ab_conv_b[j],
                             ab_q_g[j], ab_k_g[j], ab_w_out[j], rows, cols, update_ctx)
        else:
            y, yc = mixer_na(h, hc, na_w_qkv[j], na_rpb[j], na_w_out[j], update_ctx)
        x = layer_norm(ALPHA * x + g_m * y, ln_g[i, 0], ln_b[i, 0])
        if update_ctx:
            ctx = layer_norm(ALPHA * ctx + gc_m * yc, ln_g[i, 0], ln_b[i, 0])
        h = x * (1.0 + sc_f) + sh_f
        if i % 2 == 0:
            y = swiglu(h, ffn_w_gate[j], ffn_w_up[j], ffn_w_down[j])
        else:
            y = moe_swiglu(h, moe_w_router[j], moe_w_gate[j], moe_w_up[j], moe_w_down[j])
        x = layer_norm(ALPHA * x + g_f * y, ln_g[i, 1], ln_b[i, 1])
        if update_ctx:
            hc = ctx * (1.0 + scc_f) + shc_f
            if i % 2 == 0:
                yc = swiglu(hc, ffn_w_gate[j], ffn_w_up[j], ffn_w_down[j])
            else:
                yc = moe_swiglu(hc, moe_w_router[j], moe_w_gate[j], moe_w_up[j], moe_w_down[j])
            ctx = layer_norm(ALPHA * ctx + gc_f * yc, ln_g[i, 1], ln_b[i, 1])
    return x
```

```python
import contextlib
import numpy as np
import concourse.bass as bass
import concourse.mybir as mybir
from concourse.bass_utils import run_bass_kernel_spmd

F32 = mybir.dt.float32; BF16 = mybir.dt.bfloat16
AF = mybir.ActivationFunctionType
ALU = mybir.AluOpType
AX = mybir.AxisListType

D = 1024
NE = 2560; NE2 = 2816; NC = 256; NC2 = 512; NALL = 4096; NKEY = 4352
NT0 = NE + NC
NOWN = 2048
FFN = 2816; EXP = 3584; NEXP = 8
ALPHA = 4 ** 0.25
LN_EPS = 1e-5; RMS_EPS = 1e-6


class Buf:
    __slots__ = ("name", "w", "readers")
    def __init__(self, name):
        self.name = name; self.w = {}; self.readers = {}

EPOCH = 30000
NDMASEM = 10


class Prog:
    def __init__(self, nc):
        self.nc = nc
        self.es = contextlib.ExitStack()
        self.engs = ['sp', 'pe', 'act', 'dve', 'pool']
        self.streams = {k: [] for k in self.engs}
        self.cnt = {k: 0 for k in self.engs}
        self.seen = {k: {} for k in self.engs}
        self.sems = {}
        self.dma_pool = {}
        self.dma_rr = {}
        for q in ('sp', 'pool'):
            self.dma_pool[q] = [[self._sem(f"d_{q}_{i}"), 0, f"d_{q}_{i}"] for i in range(NDMASEM)]
            self.dma_rr[q] = 0
        self.nbuf = 0

    def _sem(self, name):
        s = self.es.enter_context(self.nc.semaphore(name))
        self.sems[name] = s
        return s

    def buf(self, name=None):
        self.nbuf += 1
        return Buf(name or f"b{self.nbuf}")

    def bufs(self, n, name="b"):
        return [self.buf(f"{name}{i}") for i in range(n)]

    def _eng_sem(self, eng, epoch):
        key = f"e_{eng}_{epoch}"
        if key not in self.sems:
            self._sem(key)
        return key

    def _wait(self, eng, deps):
        for key, val in deps.items():
            if self.seen[eng].get(key, 0) >= val:
                continue
            self.seen[eng][key] = val
            sem = self.sems[key]
            self.streams[eng].append(lambda e, sem=sem, val=val: e.wait_ge(sem, val))

    def _collect(self, eng, reads, writes):
        deps = {}
        def add(tok):
            if tok is None:
                return
            k, v = tok
            if eng == 'pe' and k.startswith('e_pe_'):
                return
            if deps.get(k, 0) < v:
                deps[k] = v
        for b in reads:
            for k, v in b.w.items():
                add((k, v))
        for b in writes:
            for k, v in b.w.items():
                add((k, v))
            for k, v in b.readers.items():
                add((k, v))
        return deps

    def _commit(self, tok, reads, writes, toks=None):
        toks = toks or [tok]
        for b in writes:
            b.w = {}; b.readers = {}
            for k, v in toks:
                if b.w.get(k, 0) < v:
                    b.w[k] = v
        ws = set(id(b) for b in writes)
        for b in reads:
            if id(b) in ws:
                continue
            for k, v in toks:
                if b.readers.get(k, 0) < v:
                    b.readers[k] = v

    def op(self, eng, fn, reads=(), writes=()):
        deps = self._collect(eng, reads, writes)
        self._wait(eng, deps)
        n = self.cnt[eng]; self.cnt[eng] = n + 1
        epoch, idx = divmod(n, EPOCH)
        key = self._eng_sem(eng, epoch)
        sem = self.sems[key]
        self.streams[eng].append(lambda e, fn=fn, sem=sem: fn(e).then_inc(sem, 1))
        tok = (key, idx + 1)
        self._commit(tok, reads, writes)
        return tok

    def dma(self, q, out, in_, reads=(), writes=()):
        pool = self.dma_pool[q]
        i = self.dma_rr[q]; self.dma_rr[q] = (i + 1) % len(pool)
        ent = pool[i]
        deps = self._collect(q, reads, writes)
        if ent[1] > 0 and deps.get(ent[2], 0) < ent[1]:
            deps[ent[2]] = ent[1]
        self._wait(q, deps)
        ent[1] += 16
        sem = ent[0]
        self.streams[q].append(lambda e, out=out, in_=in_, sem=sem: e.dma_start(out=out, in_=in_).then_inc(sem, 16))
        tok = (ent[2], ent[1])
        self._commit(tok, reads, writes)
        return tok

    def dma_group(self, q, pairs, reads=(), writes=()):
        base = self._collect(q, reads, writes)
        toks = []
        pool = self.dma_pool[q]
        for (out, in_) in pairs:
            i = self.dma_rr[q]; self.dma_rr[q] = (i + 1) % len(pool)
            ent = pool[i]
            deps = dict(base)
            if ent[1] > 0 and deps.get(ent[2], 0) < ent[1]:
                deps[ent[2]] = ent[1]
            self._wait(q, deps)
            ent[1] += 16
            sem = ent[0]
            self.streams[q].append(lambda e, out=out, in_=in_, sem=sem: e.dma_start(out=out, in_=in_).then_inc(sem, 16))
            toks.append((ent[2], ent[1]))
        self._commit(None, reads, writes, toks=toks)
        return toks

    def barrier(self):
        deps = {}
        for q, pool in self.dma_pool.items():
            for ent in pool:
                if ent[1] > 0:
                    deps[ent[2]] = ent[1]
        for e in self.engs:
            n = self.cnt[e]
            if n > 0:
                epoch, idx = divmod(n - 1, EPOCH)
                deps[self._eng_sem(e, epoch)] = idx + 1
        for e in self.engs:
            d = {k: v for k, v in deps.items() if not k.startswith(f"e_{e}_")}
            self._wait(e, d)

    def flush(self):
        self.barrier()
        streams = self.streams
        self.streams = {k: [] for k in self.engs}
        with self.nc.Block() as block:
            def mk(name):
                def f(e):
                    for c in streams[name]:
                        c(e)
                return f
            block.sync(mk('sp'))
            block.tensor(mk('pe'))
            block.scalar(mk('act'))
            block.vector(mk('dve'))
            block.gpsimd(mk('pool'))

    def close(self):
        self.es.close()


def build_program(stop_after=None, debug=False):
    nc = bass.Bass("TRN2", target_bir_lowering=False)
    P = Prog(nc)

    def dram_in(name, shape, dt=F32):
        return nc.dram_tensor(name, list(shape), dt, kind="ExternalInput").ap()

    def dram_scr(name, shape, dt=F32):
        if debug:
            return nc.dram_tensor(name, list(shape), dt, kind="ExternalOutput").ap()
        return nc.dram_tensor(name, list(shape), dt).ap()

    xe2 = dram_in("xe2", [NE2, D]); xall = dram_in("xall", [NALL, D]); ctx2 = dram_in("ctx2", [NC2, D])
    me2 = dram_in("me2", [128, NE2 + NC2])
    ropeq = dram_in("ropeq", [NT0, 128]); ropek = dram_in("ropek", [NKEY, 128])
    cT = dram_in("cT", [128, 16]); bmodT = dram_in("bmodT", [128, 96]); lnT = dram_in("lnT", [128, 64])
    convwT = dram_in("convwT", [128, 4 * 31]); convgbT = dram_in("convgbT", [128, 8])
    qkg = dram_in("qkg", [128, 128]); identf_d = dram_in("identf", [128, 128])
    w_mod = dram_in("w_mod", [2, D, 6 * D])
    w_in = dram_in("ab_w_in", [D, 1792]); w_out0 = dram_in("ab_w_out", [D, D])
    wg0 = dram_in("ffn_w_gate", [D, FFN]); wu0 = dram_in("ffn_w_up", [D, FFN]); wd0 = dram_in("ffn_w_down", [FFN, D])
    w_qkv = dram_in("na_w_qkv", [D, 3 * D]); w_out1 = dram_in("na_w_out", [D, D])
    nabias = dram_in("nabias", [16, 128, 14 * 64]); namask = dram_in("namask", [128, 14 * 64 * 2])
    navrow = dram_in("navrow", [128, 2 * 6 * 4])
    w_router = dram_in("moe_w_router", [D, NEXP])
    mwg = dram_in("moe_w_gate", [NEXP, D, EXP]); mwu = dram_in("moe_w_up", [NEXP, D, EXP]); mwd = dram_in("moe_w_down", [NEXP, EXP, D])
    lnrow = dram_in("lnrow", [128, 2 * D]); bmodrow = dram_in("bmodrow", [1, D])
    out_d = nc.dram_tensor("out", [NOWN, D], F32, kind="ExternalOutput").ap()

    kT_s = dram_scr("kT_s", [128, 2 * NKEY], BF16)
    va_s = dram_scr("va_s", [128, 34 * 2 * 192], BF16)
    qT_s = dram_scr("qT_s", [128, 4 * NT0], BF16)
    u_s = dram_scr("u_s", [128, 4 * (NE2 + NC2)], F32)
    ao_s = dram_scr("ao_s", [128, 8 * NT0], BF16)
    xa1_s = dram_scr("xa1_s", [128, 8 * NT0], F32)
    h1_s = dram_scr("h1_s", [128, 8 * NT0], BF16)
    xa2_s = dram_scr("xa2_s", [128, 8 * NT0], F32)
    h2_s = dram_scr("h2_s", [128, 8 * NT0], BF16)
    o1_s = dram_scr("o1_s", [128, 8 * NOWN], BF16)
    h3_s = dram_scr("h3_s", [128, 8 * NOWN], BF16)
    xa3_s = dram_scr("xa3_s", [NOWN, D], F32)
    y_s = dram_scr("y_s", [NOWN, D], F32)
    msc_s = dram_scr("msc_s", [128, 1024], F32)
    ga_s = dram_scr("ga_s", [128, 22 * NT0], BF16)
    cv_s = dram_scr("cv_s", [128, 4 * NT0], F32)
    dbg = {}

    pes = P.es
    shared = {}

    _uid = [0]

    def sb(es, name, shape, dt=F32):
        _uid[0] += 1
        return es.enter_context(nc.sbuf_tensor(f"s{_uid[0]}_{name}", list(shape), dt))

    def psum(es, name, shape, dt=F32):
        _uid[0] += 1
        return es.enter_context(nc.psum_tensor(f"p{_uid[0]}_{name}", list(shape), dt))

    def MM(out, lhsT, rhs, start, stop, reads, writes):
        return P.op('pe', lambda e: e.matmul(out, lhsT, rhs, start=start, stop=stop), reads, writes)

    def TR(out, in_, ident, reads, writes):
        return P.op('pe', lambda e: e.transpose(out, in_, ident), reads, writes)

    def ACT(out, in_, func, reads, writes, scale=None, bias=None):
        kw = {}
        if scale is not None: kw['scale'] = scale
        if bias is not None: kw['bias'] = bias
        return P.op('act', lambda e: e.activation(out=out, in_=in_, func=func, **kw), reads, writes)

    def TT(eng, out, in0, in1, op, reads, writes):
        return P.op(eng, lambda e: e.tensor_tensor(out=out, in0=in0, in1=in1, op=op), reads, writes)

    def TS(eng, out, in0, s1, s2, op0, op1, reads, writes):
        if op1 is None:
            return P.op(eng, lambda e: e.tensor_scalar(out=out, in0=in0, scalar1=s1, scalar2=None, op0=op0), reads, writes)
        return P.op(eng, lambda e: e.tensor_scalar(out=out, in0=in0, scalar1=s1, scalar2=s2, op0=op0, op1=op1), reads, writes)

    def STT(out, in0, scalar, in1, op0, op1, reads, writes):
        return P.op('dve', lambda e: e.scalar_tensor_tensor(out=out, in0=in0, scalar=scalar, in1=in1, op0=op0, op1=op1), reads, writes)

    def CP(eng, out, in_, reads, writes):
        if eng == 'act':
            return P.op(eng, lambda e: e.activation(out=out, in_=in_, func=AF.Copy), reads, writes)
        return P.op(eng, lambda e: e.tensor_copy(out=out, in_=in_), reads, writes)

    def RECIP(out, in_, reads, writes):
        return P.op('dve', lambda e: e.reciprocal(out=out, in_=in_), reads, writes)

    def MEMSET(eng, out, val, writes):
        return P.op(eng, lambda e: e.memset(out, val), (), writes)

    def bc_free(ap2d, n_outer, n_inner_bcast):
        a = ap2d.ap
        return bass.AP(tensor=ap2d.tensor, offset=ap2d.offset, ap=[list(a[0]), list(a[1]), [0, n_inner_bcast]])

    def bc_mid(ap2d, n_mid):
        a = ap2d.ap
        return bass.AP(tensor=ap2d.tensor, offset=ap2d.offset, ap=[list(a[0]), [0, n_mid], list(a[1])])

    identf = sb(pes, "identf", [128, 128]); identb = sb(pes, "identb", [128, 128], BF16)
    onesf = sb(pes, "onesf", [128, 128]); onesb = sb(pes, "onesb", [128, 128], BF16)
    msc = sb(pes, "msc", [128, 1024])
    gates = sb(pes, "gates", [128, 16 * 8])
    b_const = P.buf("const"); b_msc = P.buf("msc"); b_gates = P.buf("gates")
    P.dma('sp', identf[:], identf_d, writes=[b_const])
    P.dma('pool', identb[:], identf_d, writes=[b_const])
    MEMSET('dve', onesf[:], 1.0, [b_const])
    MEMSET('dve', onesb[:], 1.0, [b_const])

    cols = {}
    _c = [0]
    def mcol(name, n=8):
        cols[name] = _c[0]; _c[0] += n
        return cols[name]
    for i in range(2):
        for j in range(2):
            mcol(f"raw{i}{j}", 48)
    for i in range(2):
        for j in range(2):
            for nm in ("opscm", "opscf", "A1s", "A1b", "H1s", "H1b", "A2s", "A2b", "H2s", "H2b"):
                mcol(f"{nm}{i}{j}")
    mcol("ln", 64); mcol("convw", 124); mcol("convgb", 8); mcol("bmod", 96)
    def M(name, k=0, n=1):
        c = cols[name] + k
        return msc[:, c:c + n]
    RAW = {"shm": 0, "scm": 8, "gm": 16, "shf": 24, "scf": 32, "gf": 40}
    def MR(i, j, what, k=0, n=1):
        return M(f"raw{i}{j}", RAW[what] + k, n)
    def LN(i, j, gb, k=0, n=1):
        return M("ln", ((i * 2 + j) * 2 + gb) * 8 + k, n)

    def phase0(es):
        csb = sb(es, "csb", [128, 16]); scT = sb(es, "scT", [128, 16], BF16)
        wm = [sb(es, f"wm{s}", [128, 8, 1536], BF16) for s in range(2)]
        pmod = psum(es, "pmod", [128, 512])
        b_c, b_sc, b_pm = P.bufs(3); b_wm = P.bufs(2)
        P.dma('sp', csb[:], cT, writes=[b_c])
        P.dma('sp', M("ln", 0, 64), lnT, writes=[b_msc])
        P.dma('sp', M("convw", 0, 124), convwT, writes=[b_msc])
        P.dma('sp', M("convgb", 0, 8), convgbT, writes=[b_msc])
        P.dma('sp', M("bmod", 0, 96), bmodT, writes=[b_msc])
        ACT(scT[:], csb[:], AF.Silu, [b_c], [b_sc])
        it = 0
        for i in range(2):
            for grp in range(4):
                s = it % 2; it += 1
                P.dma('pool', wm[s][:], w_mod[i].rearrange("(k p) c -> p k c", p=128)[:, :, grp * 1536:(grp + 1) * 1536], writes=[b_wm[s]])
                for mm in range(12):
                    m = grp * 12 + mm
                    for k in range(8):
                        MM(pmod[:, (i * 48 + m) * 2:(i * 48 + m) * 2 + 2], wm[s][:, k, mm * 128:(mm + 1) * 128],
                           scT[:, 2 * k:2 * k + 2], k == 0, k == 7, [b_wm[s], b_sc], [b_pm])
        pv = pmod[:, 0:192].rearrange("p (i m j) -> p i m j", i=2, j=2)
        for i in range(2):
            for j in range(2):
                TT('dve', M(f"raw{i}{j}", 0, 48), pv[:, i, :, j], M("bmod", i * 48, 48), ALU.add, [b_pm, b_msc], [b_msc])
        for i in range(2):
            for j in range(2):
                TS('dve', M(f"opscm{i}{j}", 0, 8), MR(i, j, "scm", 0, 8), 1.0, None, ALU.add, None, [b_msc], [b_msc])
                TS('dve', M(f"opscf{i}{j}", 0, 8), MR(i, j, "scf", 0, 8), 1.0, None, ALU.add, None, [b_msc], [b_msc])
        for i in range(2):
            for j in range(2):
                TS('dve', M(f"A1s{i}{j}", 0, 8), LN(i, 0, 0, 0, 8), ALPHA, None, ALU.mult, None, [b_msc], [b_msc])
                TS('dve', M(f"A1b{i}{j}", 0, 8), LN(i, 0, 1, 0, 8), ALPHA, None, ALU.mult, None, [b_msc], [b_msc])
                TT('dve', M(f"H1s{i}{j}", 0, 8), LN(i, 0, 0, 0, 8), M(f"opscf{i}{j}", 0, 8), ALU.mult, [b_msc], [b_msc])
                TT('dve', M(f"H1b{i}{j}", 0, 8), LN(i, 0, 1, 0, 8), M(f"opscf{i}{j}", 0, 8), ALU.mult, [b_msc], [b_msc])
                TT('dve', M(f"H1b{i}{j}", 0, 8), M(f"H1b{i}{j}", 0, 8), MR(i, j, "shf", 0, 8), ALU.add, [b_msc], [b_msc])
        for j in range(2):
            TS('dve', M(f"A2s0{j}", 0, 8), LN(0, 1, 0, 0, 8), ALPHA, None, ALU.mult, None, [b_msc], [b_msc])
            TS('dve', M(f"A2b0{j}", 0, 8), LN(0, 1, 1, 0, 8), ALPHA, None, ALU.mult, None, [b_msc], [b_msc])
            TT('dve', M(f"H2s0{j}", 0, 8), LN(0, 1, 0, 0, 8), M(f"opscm1{j}", 0, 8), ALU.mult, [b_msc], [b_msc])
            TT('dve', M(f"H2b0{j}", 0, 8), LN(0, 1, 1, 0, 8), M(f"opscm1{j}", 0, 8), ALU.mult, [b_msc], [b_msc])
            TT('dve', M(f"H2b0{j}", 0, 8), M(f"H2b0{j}", 0, 8), MR(1, j, "shm", 0, 8), ALU.add, [b_msc], [b_msc])
        if debug:
            P.dma('sp', msc_s, msc[:], reads=[b_msc])

    def load_T(src_rows, xt, b_xt, ptr, b_ptr):
        P.dma('sp', xt[:], src_rows, writes=[b_xt])
        for k in range(8):
            TR(ptr[:, k * 128:(k + 1) * 128], xt[:, k * 128:(k + 1) * 128], identf[:], [b_xt, b_const], [b_ptr])

    def rms_rope(src, nh, gain_ap, rope_t, tmp, b_tmp, reads, dst, dst_writes, dup=False):
        n = nh * 64
        B = b_tmp
        srcv = src.rearrange("p (h d) -> p h d", d=64)
        ACT(tmp['sq'][:, :n], src, AF.Square, reads, [B['sq']])
        P.op('dve', lambda e: e.tensor_reduce(out=tmp['ss'][:, :nh], in_=tmp['sq'][:, :n].rearrange("p (h d) -> p h d", d=64),
                                              axis=AX.X, op=ALU.add), [B['sq']], [B['ss']])
        TS('dve', tmp['ss'][:, :nh], tmp['ss'][:, :nh], 1.0 / 64, RMS_EPS, ALU.mult, ALU.add, [B['ss']], [B['ss']])
        ACT(tmp['rs'][:, :nh], tmp['ss'][:, :nh], AF.Sqrt, [B['ss']], [B['ss']])
        RECIP(tmp['rs'][:, :nh], tmp['rs'][:, :nh], [B['ss']], [B['ss']])
        knv = tmp['kn'][:, :n].rearrange("p (h d) -> p h d", d=64)
        TT('dve', knv, srcv, bc_free(tmp['rs'][:, :nh], nh, 64), ALU.mult, reads + [B['ss']], [B['kn']])
        TT('dve', knv, knv, bc_mid(gain_ap, nh), ALU.mult, [B['kn'], b_const], [B['kn']])
        kn5 = tmp['kn'][:, :n].rearrange("p (h g s d) -> p h g s d", g=2, s=2, d=16)
        t25 = tmp['t2'][:, :n].rearrange("p (h g s d) -> p h g s d", g=2, s=2, d=16)
        sin4 = rope_t[:, 64:128].rearrange("p (g s d) -> p g s d", g=2, s=2, d=16)
        for s in range(2):
            for g in range(2):
                sa = sin4[:, g, s, :]
                sinb = bass.AP(tensor=sa.tensor, offset=sa.offset, ap=[list(sa.ap[0]), [0, nh], list(sa.ap[1])])
                TT('pool', t25[:, :, g, s, :], kn5[:, :, g, 1 - s, :], sinb, ALU.mult, [B['kn']] + reads, [B['t2']])
        t1v = tmp['t1'][:, :n].rearrange("p (h d) -> p h d", d=64)
        TT('dve', t1v, knv, bc_mid(rope_t[:, 0:64], nh), ALU.mult, [B['kn']] + reads, [B['t1']])
        t2v = tmp['t2'][:, :n].rearrange("p (h d) -> p h d", d=64)
        if dup:
            for dd in range(2):
                TT('dve', dst[:, :, dd, :], t1v, t2v, ALU.add, [B['t1'], B['t2']], dst_writes)
        else:
            TT('dve', dst, t1v, t2v, ALU.add, [B['t1'], B['t2']], dst_writes)

    def mk_tmp(es, pfx):
        t = {'sq': sb(es, pfx + "sq", [128, 512]), 'ss': sb(es, pfx + "ss", [128, 8]), 'rs': sb(es, pfx + "rs", [128, 8]),
             'kn': sb(es, pfx + "kn", [128, 512]), 't1': sb(es, pfx + "t1", [128, 512]), 't2': sb(es, pfx + "t2", [128, 512])}
        b = {k: P.buf() for k in ('sq', 'ss', 'kn', 't1', 't2')}
        return t, b

    def phaseA(es, post_init=None):
        xt = [sb(es, f"xt{s}", [128, D]) for s in range(2)]
        hT = [sb(es, f"hT{s}", [128, 8, 128], BF16) for s in range(2)]
        rk = [sb(es, f"rk{s}", [128, 128]) for s in range(2)]
        wkv = sb(es, "wkv", [128, 8, 256], BF16)
        qkg_t = sb(es, "qkg_t", [128, 128])
        kdup = [sb(es, f"kdup{s}", [128, 256], BF16) for s in range(2)]
        kT = sb(es, "kT", [128, 2 * NKEY], BF16)
        va = sb(es, "va", [128, 34 * 2 * 192], BF16)
        tmps = [mk_tmp(es, f"a{q}") for q in range(2)]
        ptr = [psum(es, f"ptr{s}", [128, 1024]) for s in range(2)]
        pkv = [psum(es, f"pkv{s}", [128, 512]) for s in range(2)]
        pkt = [psum(es, f"pkt{s}", [128, 256], BF16) for s in range(2)]
        b_xt = P.bufs(2); b_hT = P.bufs(2); b_rk = P.bufs(2); b_kd = P.bufs(2); b_ptr = P.bufs(2); b_pkv = P.bufs(2); b_pkt = P.bufs(2)
        b_w, b_kT, b_va = P.bufs(3)
        P.dma('pool', wkv[:], w_in.rearrange("(k p) c -> p k c", p=128)[:, :, 1536:1792], writes=[b_w])
        if post_init:
            post_init()
        P.dma('sp', qkg_t[:], qkg, writes=[b_const])
        MEMSET('pool', va[:], 1.0, [b_va])
        kTv = kT[:].rearrange("p (g t) -> p g t", g=2)
        vav = va[:].rearrange("p (t g c) -> p t g c", g=2, c=192)
        def st1(t):
            s = t % 2
            j = 1 if t < 2 else 0
            src = ctx2[128 + 128 * t:256 + 128 * t, :] if t < 2 else xall[(t - 2) * 128:(t - 1) * 128, :]
            load_T(src, xt[s], b_xt[s], ptr[s], b_ptr[s])
            P.dma('sp', rk[s][:], ropek[t * 128:(t + 1) * 128, :], writes=[b_rk[s]])
            for k in range(8):
                ACT(hT[s][:, k, :], ptr[s][:, k * 128:(k + 1) * 128], AF.Identity, [b_ptr[s], b_msc], [b_hT[s]],
                    scale=M(f"opscm0{j}", k), bias=MR(0, j, "shm", k))

        def st2(t):
            s = t % 2
            for k in range(8):
                MM(pkv[s][:, 0:256], hT[s][:, k, :], wkv[:, k, :], k == 0, k == 7, [b_hT[s], b_w], [b_pkv[s]])
            kd4 = kdup[s][:].rearrange("p (g dd d) -> p g dd d", g=2, dd=2)
            rms_rope(pkv[s][:, 0:128], 2, qkg_t[:, 64:128], rk[s], tmps[s][0], tmps[s][1], [b_pkv[s], b_rk[s]], kd4, [b_kd[s]], dup=True)
            for off in (0, 128):
                CP('dve', vav[:, t, :, off:off + 64], pkv[s][:, 128:256].rearrange("p (g d) -> p g d", g=2),
                   [b_pkv[s], tmps[s][1]['kn']], [b_va])

        def st3(t):
            s = t % 2
            for g in range(2):
                TR(pkt[s][:, g * 128:(g + 1) * 128], kdup[s][:, g * 128:(g + 1) * 128], identb[:], [b_kd[s], b_const], [b_pkt[s]])
            CP('act', kTv[:, :, t * 128:(t + 1) * 128], pkt[s][:].rearrange("p (g t) -> p g t", g=2), [b_pkt[s]], [b_kT])

        for t in range(34 + 2):
            if t < 34:
                st1(t)
            if 1 <= t <= 34:
                st2(t - 1)
            if t >= 2:
                st3(t - 2)
        P.dma('sp', kT_s, kT[:], reads=[b_kT])
        P.dma('sp', va_s, va[:], reads=[b_va])

    def phaseB(es, pre=None):
        xt = [sb(es, f"xt{s}", [128, D]) for s in range(2)]
        hT = [sb(es, f"hT{s}", [128, 8, 512], BF16) for s in range(2)]
        rq = [sb(es, f"rq{s}", [128, 128]) for s in range(8)]
        wA = pre['wA'][0] if pre else sb(es, "wA", [128, 8, 1536], BF16)
        qkg_t = sb(es, "qkg_t", [128, 128])
        mk = [sb(es, f"mk{s}", [128, 512]) for s in range(2)]
        sg = [sb(es, f"sg{s}", [128, 512]) for s in range(2)]
        ut = [sb(es, f"ut{s}", [128, 4, 512]) for s in range(2)]
        qr = [sb(es, f"qr{s}", [128, 512], BF16) for s in range(2)]
        qTt = [sb(es, f"qTt{s}", [128, 4, 128], BF16) for s in range(2)]
        tmps = [mk_tmp(es, f"b{q}") for q in range(2)]
        ptr = psum(es, "ptr", [128, 1024])
        pa = [psum(es, f"pa{s}", [128, 512]) for s in range(4)]
        pq = psum(es, "pq", [128, 512])
        pqt = psum(es, "pqt", [128, 512], BF16)
        b_xt = P.bufs(2); b_hT = P.bufs(2); b_rq = P.bufs(8); b_mk = P.bufs(2); b_sg = P.bufs(2); b_ut = P.bufs(2)
        b_qr = P.bufs(2); b_qTt = P.bufs(2); b_pa = P.bufs(4)
        b_w, b_ptr, b_pq, b_pqt = P.bufs(4)
        if pre:
            b_w = pre['wA'][1]
        else:
            P.dma('pool', wA[:], w_in.rearrange("(k p) c -> p k c", p=128)[:, :, 0:1536], writes=[b_w])
        P.dma('sp', qkg_t[:], qkg, writes=[b_const])
        uv = u_s.rearrange("p (c t) -> p c t", c=4)
        qv = qT_s.rearrange("p (c t) -> p c t", c=4)
        chunks = []
        for ci in range(6):
            n = 512 if ci < 5 else 256
            chunks.append((0, xe2, ci * 512, n, ci * 512))
        chunks.append((1, ctx2, 0, 512, NE2))
        tcnt = {'i': 0}

        def qtile_t0(j, grow):
            if j == 0:
                if grow < 128 or grow >= 128 + NE:
                    return None
                return grow - 128
            if grow < 128 or grow >= 128 + NC:
                return None
            return NE + (grow - 128)

        pend = []

        def q_finish(t0, s3):
            for pr in range(4):
                TR(pqt[:, pr * 128:(pr + 1) * 128], qr[s3][:, pr * 128:(pr + 1) * 128], identb[:], [b_qr[s3], b_const], [b_pqt])
            CP('act', qTt[s3][:], pqt[:].rearrange("p (c t) -> p c t", c=4), [b_pqt], [b_qTt[s3]])
            P.dma('sp', qv[:, :, t0:t0 + 128], qTt[s3][:], reads=[b_qTt[s3]])

        def prep(cidx):
            (j, src, r0, n, ucol) = chunks[cidx]
            s = cidx % 2
            nt = n // 128
            P.dma('sp', mk[s][:, :n], me2[:, ucol:ucol + n], writes=[b_mk[s]])
            for ti in range(nt):
                t0_ = qtile_t0(j, r0 + ti * 128)
                if t0_ is not None:
                    sr = (t0_ // 128) % 8
                    P.dma('sp', rq[sr][:], ropeq[t0_:t0_ + 128, :], writes=[b_rq[sr]])
            for ti in range(nt):
                s2 = tcnt['i'] % 2; tcnt['i'] += 1
                load_T(src[r0 + ti * 128:r0 + (ti + 1) * 128, :], xt[s2], b_xt[s2], ptr, b_ptr)
                for k in range(8):
                    ACT(hT[s][:, k, ti * 128:(ti + 1) * 128], ptr[:, k * 128:(k + 1) * 128], AF.Identity, [b_ptr, b_msc], [b_hT[s]],
                        scale=M(f"opscm0{j}", k), bias=MR(0, j, "shm", k))

        prep(0)
        for cidx, (j, src, r0, n, ucol) in enumerate(chunks):
            s = cidx % 2
            nt = n // 128
            if cidx + 1 < len(chunks):
                prep(cidx + 1)
            def do_q(ti):
                t0 = qtile_t0(j, r0 + ti * 128)
                s3 = (t0 // 128) % 2
                sr = (t0 // 128) % 8
                for k in range(8):
                    MM(pq[:, :], hT[s][:, k, ti * 128:(ti + 1) * 128], wA[:, k, 1024:1536], k == 0, k == 7, [b_hT[s], b_w], [b_pq])
                rms_rope(pq[:, :], 8, qkg_t[:, 0:64], rq[sr], tmps[s3][0], tmps[s3][1], [b_pq, b_rq[sr]],
                         qr[s3][:].rearrange("p (h d) -> p h d", d=64), [b_qr[s3]])
                if pend:
                    q_finish(*pend.pop())
                pend.append((t0, s3))

            qtiles = [ti for ti in range(nt) if qtile_t0(j, r0 + ti * 128) is not None]
            for c in range(4):
                if c < len(qtiles):
                    do_q(qtiles[c])
                pv_, pg_ = pa[(2 * c) % 4], pa[(2 * c + 1) % 4]
                bv_, bg_ = b_pa[(2 * c) % 4], b_pa[(2 * c + 1) % 4]
                for k in range(8):
                    MM(pv_[:, :n], wA[:, k, c * 128:(c + 1) * 128], hT[s][:, k, :n], k == 0, k == 7, [b_w, b_hT[s]], [bv_])
                for k in range(8):
                    MM(pg_[:, :n], wA[:, k, 512 + c * 128:512 + (c + 1) * 128], hT[s][:, k, :n], k == 0, k == 7, [b_w, b_hT[s]], [bg_])
                ss_ = c % 2
                ACT(sg[ss_][:, :n], pg_[:, :n], AF.Sigmoid, [bg_], [b_sg[ss_]])
                TT('pool', sg[ss_][:, :n], sg[ss_][:, :n], mk[s][:, :n], ALU.mult, [b_sg[ss_], b_mk[s]], [b_sg[ss_]])
                TT('dve', ut[s][:, c, :n], pv_[:, :n], sg[ss_][:, :n], ALU.mult, [bv_, b_sg[ss_]], [b_ut[s]])
            P.dma('sp', uv[:, :, ucol:ucol + n], ut[s][:, :, :n], reads=[b_ut[s]])
        while pend:
            q_finish(*pend.pop())

    def ln_stats(vchunks, n, nfeat, st, b_st, ps1, ps2, b_ps1, b_ps2, reads):
        nch = len(vchunks)
        for c, v in enumerate(vchunks):
            MM(ps1[:, :n], onesf[:], v, c == 0, c == nch - 1, reads + [b_const], [b_ps1])
        for c, v in enumerate(vchunks):
            q = c % 2
            ACT(st['sqb'][q][:, :n], v, AF.Square, reads, [b_st['sq'][q]])
            MM(ps2[:, :n], onesb[:], st['sqb'][q][:, :n], c == 0, c == nch - 1, [b_st['sq'][q], b_const], [b_ps2])
        ACT(st['mean'][:, :n], ps1[:, :n], AF.Copy, [b_ps1], [b_st['m']], scale=1.0 / nfeat)
        TT('dve', st['msq'][:, :n], st['mean'][:, :n], st['mean'][:, :n], ALU.mult, [b_st['m']], [b_st['r']])
        STT(st['rstd'][:, :n], ps2[:, :n], 1.0 / nfeat, st['msq'][:, :n], ALU.mult, ALU.subtract, [b_ps2, b_st['r']], [b_st['r']])
        TS('dve', st['rstd'][:, :n], st['rstd'][:, :n], 0.0, None, ALU.max, None, [b_st['r']], [b_st['r']])
        ACT(st['rstd'][:, :n], st['rstd'][:, :n], AF.Sqrt, [b_st['r']], [b_st['r']], bias=st['eps'][:, 0:1])
        RECIP(st['rstd'][:, :n], st['rstd'][:, :n], [b_st['r']], [b_st['r']])

    def mk_st(es, pfx, eps):
        st = {'sqb': [sb(es, pfx + f"sqb{q}", [128, 512], BF16) for q in range(2)], 'mean': sb(es, pfx + "mean", [128, 512]),
              'msq': sb(es, pfx + "msq", [128, 512]), 'rstd': sb(es, pfx + "rstd", [128, 512]), 'eps': sb(es, pfx + "eps", [128, 1])}
        b_st = {'sq': P.bufs(2), 'm': P.buf(), 'r': P.buf()}
        MEMSET('dve', st['eps'][:], eps, [b_st['r']])
        return st, b_st

    def phaseC_gen(es):
        ub = [sb(es, f"ub{s}", [128, 4, 512 + 32]) for s in range(2)]
        acc = [sb(es, f"acc{s}", [128, 4, 512]) for s in range(2)]
        b_ub = P.bufs(2); b_acc = [P.bufs(4) for _ in range(2)]
        uv = u_s.rearrange("p (c t) -> p c t", c=4)
        cvv = cv_s.rearrange("p (c t) -> p c t", c=4)
        chunks = [(128 + ci * 512, 512, ci * 512) for ci in range(5)] + [(NE2 + 128, 256, NE)]
        for ci, (ucol, n, t0) in enumerate(chunks):
            s = ci % 2
            P.dma('pool', ub[s][:, :, :n + 32], uv[:, :, ucol - 16:ucol + n + 16], writes=[b_ub[s]])
            yield
            for k in range(31):
                for c in range(4):
                    src = ub[s][:, c, 1 + k:1 + k + n]
                    w = M("convw", c * 31 + k)
                    if k == 0:
                        TS('dve', acc[s][:, c, :n], src, w, None, ALU.mult, None, [b_ub[s], b_msc], [b_acc[s][c]])
                    else:
                        STT(acc[s][:, c, :n], src, w, acc[s][:, c, :n], ALU.mult, ALU.add, [b_ub[s], b_msc, b_acc[s][c]], [b_acc[s][c]])
                    yield
            P.dma('pool', cvv[:, :, t0:t0 + n], acc[s][:, :, :n], reads=list(b_acc[s]))
            yield

    def phaseD(es, post_init=None):
        NB = 3; L = 2
        cgen = phaseC_gen(es)
        kTz = [sb(es, f"kTz{h}", [128, 2 * NKEY], BF16) for h in range(2)]
        va = sb(es, "va", [128, 34 * 2 * 192], BF16)
        qT = [sb(es, f"qT{s}", [128, 4, 512], BF16) for s in range(2)]
        pT = [sb(es, f"pT{s}", [128, 2, 512], BF16) for s in range(NB)]
        rs = [sb(es, f"rs{s}", [128, 512]) for s in range(2)]
        ob = [sb(es, f"ob{s}", [128, 4, 512], BF16) for s in range(2)]
        pss = [psum(es, f"pss{s}", [128, 1024]) for s in range(NB)]
        po = [psum(es, f"po{s}", [128, 512]) for s in range(2)]
        b_qT = P.bufs(2); b_pT = P.bufs(NB); b_rs = P.bufs(2); b_ob = P.bufs(2); b_pss = P.bufs(NB); b_po = P.bufs(2)
        b_kT, b_va = P.bufs(2)
        P.dma_group('sp', [(kTz[0][:], kT_s), (kTz[1][:], kT_s)], writes=[b_kT])
        P.dma('sp', va[:], va_s, writes=[b_va])
        if post_init:
            post_init()
        MEMSET('dve', kTz[0][64:128, :], 0.0, [b_kT])
        MEMSET('pool', kTz[1][0:64, :], 0.0, [b_kT])
        kTv = [kTz[h][:].rearrange("p (g t) -> p g t", g=2) for h in range(2)]
        vav = va[:].rearrange("p (t g c) -> p t g c", g=2, c=192)
        qv = qT_s.rearrange("p (c t) -> p c t", c=4)
        aov = ao_s.rearrange("p (c t) -> p c t", c=8)
        chunks = [(ci * 512, 512, 34) for ci in range(5)] + [(NE, 256, 2)]
        items = []
        for ci, (t0, n, nkc) in enumerate(chunks):
            for pr in range(4):
                for kc in range(nkc):
                    items.append((ci, t0, n, nkc, pr, kc))

        def qk_exp(i):
            ci, t0, n, nkc, pr, kc = items[i]
            s = ci % 2; g = pr // 2; s3 = i % NB
            if pr == 0 and kc == 0:
                P.dma('sp', qT[s][:, :, :n], qv[:, :, t0:t0 + n], writes=[b_qT[s]])
            for hh in range(2):
                MM(pss[s3][:, hh * 512:hh * 512 + n], kTv[hh][:, g, kc * 128:(kc + 1) * 128], qT[s][:, pr, :n], True, True,
                   [b_kT, b_qT[s]], [b_pss[s3]])
            ACT(pT[s3][:, :, :n], pss[s3][:].rearrange("p (h q) -> p h q", h=2)[:, :, :n], AF.Exp, [b_pss[s3]], [b_pT[s3]])

        def pv(i):
            ci, t0, n, nkc, pr, kc = items[i]
            s = ci % 2; g = pr // 2; s3 = i % NB
            for hh in range(2):
                lo = 0 if hh == 0 else 64
                MM(po[hh][:, :n], vav[:, kc, g, lo:lo + 128], pT[s3][:, hh, :n], kc == 0, kc == nkc - 1, [b_va, b_pT[s3]], [b_po[hh]])
            if kc == nkc - 1:
                for hh in range(2):
                    ob_, sm_ = (0, 64) if hh == 0 else (64, 0)
                    RECIP(rs[hh][ob_:ob_ + 64, :n], po[hh][sm_:sm_ + 64, :n], [b_po[hh]], [b_rs[hh]])
                    TT('dve', ob[s][ob_:ob_ + 64, pr, :n], po[hh][ob_:ob_ + 64, :n], rs[hh][ob_:ob_ + 64, :n], ALU.mult,
                       [b_po[hh], b_rs[hh]], [b_ob[s]])
                if pr == 3:
                    P.dma('sp', aov[:, 4:8, t0:t0 + n], ob[s][:, :, :n], reads=[b_ob[s]])

        for i in range(len(items) + L):
            if i < len(items):
                qk_exp(i)
            if i >= L:
                pv(i - L)
            for _ in range(2 if i % 4 == 0 else 1):
                next(cgen, None)
        for _ in cgen:
            pass

    def resid_ln(vt, b_vt, n, st, b_st, ps1, ps2, b_ps1, b_ps2, xc, b_xc, out_xa, out_h, b_oxa, b_oh, names, o=0):
        ln_stats([vt[:, m, o:o + n] for m in range(8)], n, 1024, st, b_st, ps1, ps2, b_ps1, b_ps2, [b_vt])
        resid_ln_tail(vt, b_vt, n, st, b_st, xc, b_xc, out_xa, out_h, b_oxa, b_oh, names, o)

    def resid_ln_tail(vt, b_vt, n, st, b_st, xc, b_xc, out_xa, out_h, b_oxa, b_oh, names, o=0):
        As, Ab, Hs, Hb = names
        TT('dve', xc[:, :, :n], vt[:, :, o:o + n], bc_mid(st['mean'][:, :n], 8), ALU.subtract, [b_vt, b_st['m']], [b_xc])
        TT('dve', xc[:, :, :n], xc[:, :, :n], bc_mid(st['rstd'][:, :n], 8), ALU.mult, [b_xc, b_st['r']], [b_xc])
        for m in range(8):
            ACT(out_xa[:, m, :n], xc[:, m, :n], AF.Identity, [b_xc, b_msc], [b_oxa], scale=M(As, m), bias=M(Ab, m))
            ACT(out_h[:, m, :n], xc[:, m, :n], AF.Identity, [b_xc, b_msc], [b_oh], scale=M(Hs, m), bias=M(Hb, m))

    def phaseE1(es, pre=None):
        xt = [sb(es, f"xt{s}", [128, D]) for s in range(4)]
        wo = pre['wo'][0] if pre else sb(es, "wo", [128, 8, D], BF16)
        ao = [sb(es, f"ao{s}", [128, 8, 512], BF16) for s in range(2)]
        xa = [sb(es, f"xa{s}", [128, 8, 512]) for s in range(2)]
        vt = [sb(es, f"vt{s}", [128, 8, 512]) for s in range(2)]
        oxa = [sb(es, "oxa0", [128, 8, 512])] * 2
        oh = [sb(es, f"oh{s}", [128, 8, 512], BF16) for s in range(2)]
        xc = sb(es, "xcb", [128, 8, 512])
        st, b_st = mk_st(es, "e", LN_EPS)
        cvt = [sb(es, f"cvt{s}", [128, 4, 512]) for s in range(2)]
        xc2 = xc[:, 0:4, :]
        st2, b_st2 = mk_st(es, "e2", LN_EPS)
        b_cvt = P.bufs(2)
        cvv = cv_s.rearrange("p (c t) -> p c t", c=4)
        ptr = psum(es, "ptr", [128, 1024]); py = [psum(es, f"py{s}", [128, 512]) for s in range(2)]
        ps1 = psum(es, "ps1", [128, 512]); ps2 = psum(es, "ps2", [128, 512])
        b_xt = P.bufs(4); b_ao = P.bufs(2); b_xa = P.bufs(2); b_vt = P.bufs(2); b_oxa = [P.buf()] * 2; b_oh = P.bufs(2); b_xc = P.buf(); b_py = P.bufs(2)
        b_w, b_ptr, b_ps1, b_ps2 = P.bufs(4)
        if pre:
            b_w = pre['wo'][1]
        else:
            P.dma('pool', wo[:], w_out0.rearrange("(k p) c -> p k c", p=128), writes=[b_w])
        aov = ao_s.rearrange("p (c t) -> p c t", c=8)
        xav = xa1_s.rearrange("p (c t) -> p c t", c=8)
        hv = h1_s.rearrange("p (c t) -> p c t", c=8)
        chunks = [(0, xe2, 128 + ci * 512, 512, ci * 512) for ci in range(5)] + [(1, ctx2, 128, 256, NE)]
        def loads(ci):
            (j, src, r0, n, t0) = chunks[ci]
            s = ci % 2
            P.dma('sp', ao[s][:, 4:8, :n], aov[:, 4:8, t0:t0 + n], writes=[b_ao[s]])
            P.dma('sp', cvt[s][:, :, :n], cvv[:, :, t0:t0 + n], writes=[b_cvt[s]])
            for ti in range(n // 128):
                P.dma('sp', xt[ti][:], src[r0 + ti * 128:r0 + (ti + 1) * 128, :], writes=[b_xt[ti]])

        def conv_ln(ci):
            (j, src, r0, n, t0) = chunks[ci]
            s = ci % 2
            b_xc2 = b_xc
            ln_stats([cvt[s][:, c, :n] for c in range(4)], n, 512, st2, b_st2, ps1, ps2, b_ps1, b_ps2, [b_cvt[s]])
            TT('dve', xc2[:, :, :n], cvt[s][:, :, :n], bc_mid(st2['mean'][:, :n], 4), ALU.subtract, [b_cvt[s], b_st2['m']], [b_xc2])
            TT('dve', xc2[:, :, :n], xc2[:, :, :n], bc_mid(st2['rstd'][:, :n], 4), ALU.mult, [b_xc2, b_st2['r']], [b_xc2])
            for c in range(4):
                ACT(ao[s][:, c, :n], xc2[:, c, :n], AF.Silu, [b_xc2, b_msc], [b_ao[s]], scale=M("convgb", c), bias=M("convgb", 4 + c))

        def trans(ci):
            (j, src, r0, n, t0) = chunks[ci]
            s = ci % 2
            for ti in range(n // 128):
                for k in range(8):
                    TR(ptr[:, k * 128:(k + 1) * 128], xt[ti][:, k * 128:(k + 1) * 128], identf[:], [b_xt[ti], b_const], [b_ptr])
                ACT(xa[s][:, :, ti * 128:(ti + 1) * 128], ptr[:].rearrange("p (k t) -> p k t", k=8), AF.Copy, [b_ptr], [b_xa[s]], scale=ALPHA)

        loads(0); trans(0); conv_ln(0); loads(1); conv_ln(1)
        for ci, (j, src, r0, n, t0) in enumerate(chunks):
            s = ci % 2
            for m in range(8):
                sp_ = m % 2
                for k in range(8):
                    MM(py[sp_][:, :n], wo[:, k, m * 128:(m + 1) * 128], ao[s][:, k, :n], k == 0, k == 7, [b_w, b_ao[s]], [b_py[sp_]])
                STT(vt[s][:, m, :n], py[sp_][:, :n], MR(0, j, "gm", m), xa[s][:, m, :n], ALU.mult, ALU.add,
                    [b_py[sp_], b_msc, b_xa[s]], [b_vt[s]])
            ln_stats([vt[s][:, m, :n] for m in range(8)], n, 1024, st, b_st, ps1, ps2, b_ps1, b_ps2, [b_vt[s]])
            if ci + 1 < len(chunks):
                trans(ci + 1)
            if ci + 2 < len(chunks):
                loads(ci + 2)
            resid_ln_tail(vt[s], b_vt[s], n, st, b_st, xc, b_xc, oxa[s], oh[s], b_oxa[s], b_oh[s],
                          (f"A1s0{j}", f"A1b0{j}", f"H1s0{j}", f"H1b0{j}"))
            P.dma('sp', xav[:, :, t0:t0 + n], oxa[s][:, :, :n], reads=[b_oxa[s]])
            P.dma('sp', hv[:, :, t0:t0 + n], oh[s][:, :, :n], reads=[b_oh[s]])
            if ci + 2 < len(chunks):
                conv_ln(ci + 2)

    def phaseE2a(es, post_init=None):
        hb = sb(es, "hb", [128, 8, NT0], BF16)
        wgu = [sb(es, f"wgu{s}", [128, 2, 8, 512], BF16) for s in range(2)]
        sg = [sb(es, f"sg{s}", [128, 512]) for s in range(2)]
        gat = [sb(es, f"gat{s}", [128, 4, 512], BF16) for s in range(3)]
        pg = [psum(es, f"pg{s}", [128, 512]) for s in range(3)]; pu = [psum(es, f"pu{s}", [128, 512]) for s in range(3)]
        b_wgu = P.bufs(2); b_sg = P.bufs(2); b_gat = P.bufs(3); b_pg = P.bufs(3); b_pu = P.bufs(3)
        shared['e2'] = (pg, pu, b_pg, b_pu)
        chunks = [(ci * 512, 512) for ci in range(5)] + [(NE, 256)]
        b_hb = P.bufs(len(chunks))
        hv = h1_s.rearrange("p (c t) -> p c t", c=8)
        gav = ga_s.rearrange("p (c t) -> p c t", c=22)
        for ci, (t0, n) in enumerate(chunks):
            P.dma('sp', hb[:, :, t0:t0 + n], hv[:, :, t0:t0 + n], writes=[b_hb[ci]])
        groups = [(0, 4), (4, 4), (8, 4), (12, 4), (16, 4), (20, 2)]
        isg = 0; iq = 0
        for gi, (c0, ncg) in enumerate(groups):
            s = gi % 2
            P.dma_group('pool', [(wgu[s][:, 0, :, :ncg * 128], wg0.rearrange("(k p) c -> p k c", p=128)[:, :, c0 * 128:(c0 + ncg) * 128]),
                                 (wgu[s][:, 1, :, :ncg * 128], wu0.rearrange("(k p) c -> p k c", p=128)[:, :, c0 * 128:(c0 + ncg) * 128])],
                        writes=[b_wgu[s]])
            if gi == 1 and post_init:
                post_init()
            for ci, (t0, n) in enumerate(chunks):
                q = iq % 3; iq += 1
                for cc in range(ncg):
                    sp_ = isg % 3; ss_ = isg % 2; isg += 1
                    for k in range(8):
                        MM(pg[sp_][:, :n], wgu[s][:, 0, k, cc * 128:(cc + 1) * 128], hb[:, k, t0:t0 + n], k == 0, k == 7, [b_wgu[s], b_hb[ci]], [b_pg[sp_]])
                    for k in range(8):
                        MM(pu[sp_][:, :n], wgu[s][:, 1, k, cc * 128:(cc + 1) * 128], hb[:, k, t0:t0 + n], k == 0, k == 7, [b_wgu[s], b_hb[ci]], [b_pu[sp_]])
                    ACT(sg[ss_][:, :n], pg[sp_][:, :n], AF.Silu, [b_pg[sp_]], [b_sg[ss_]])
                    TT('dve', gat[q][:, cc, :n], pu[sp_][:, :n], sg[ss_][:, :n], ALU.mult, [b_pu[sp_], b_sg[ss_]], [b_gat[q]])
                P.dma('sp', gav[:, c0:c0 + ncg, t0:t0 + n], gat[q][:, :ncg, :n], reads=[b_gat[q]])

    def phaseE2b(es, pre=None):
        wd = pre['wd'][0] if pre else sb(es, "wd", [128, 22, D], BF16)
        gab = [sb(es, f"gab{s}", [128, 22, 512], BF16) for s in range(2)]
        xab = [sb(es, f"xab{s}", [128, 8, 512]) for s in range(2)]
        oh = [sb(es, f"oh{s}", [128, 8, 512], BF16) for s in range(2)]
        xc = sb(es, "xcb", [128, 8, 512])
        st, b_st = mk_st(es, "f", LN_EPS)
        py = [psum(es, f"py{s}", [128, 512]) for s in range(3)]
        ps1 = psum(es, "ps1", [128, 512]); ps2 = psum(es, "ps2", [128, 512])
        b_gab = P.bufs(2); b_xab = P.bufs(2); b_oh = P.bufs(2); b_py = P.bufs(3); b_xc = P.buf()
        b_ps1, b_ps2 = P.bufs(2)
        if pre:
            b_wd = pre['wd'][1]
        else:
            b_wd = P.buf()
            P.dma_group('pool', [(wd[:, 0:11, :], wd0.rearrange("(c p) m -> p c m", p=128)[:, 0:11, :]),
                                 (wd[:, 11:22, :], wd0.rearrange("(c p) m -> p c m", p=128)[:, 11:22, :])], writes=[b_wd])
        gav = ga_s.rearrange("p (c t) -> p c t", c=22)
        xav = xa1_s.rearrange("p (c t) -> p c t", c=8)
        hv2 = h2_s.rearrange("p (c t) -> p c t", c=8)
        xav2 = xa2_s.rearrange("p (c t) -> p c t", c=8)
        chunks = [(ci * 512, 512) for ci in range(5)] + [(NE, 256)]
        NCH = len(chunks)
        cpy = {'i': 0}

        def loads(ci):
            t0, n = chunks[ci]; s = ci % 2
            P.dma('sp', gab[s][:, :, :n], gav[:, :, t0:t0 + n], writes=[b_gab[s]])
            P.dma('sp', xab[s][:, :, :n], xav[:, :, t0:t0 + n], writes=[b_xab[s]])

        def mm_stats(ci):
            t0, n = chunks[ci]; s = ci % 2
            j = 1 if t0 >= NE else 0
            for m in range(8):
                sp_ = cpy['i'] % 3; cpy['i'] += 1
                for c in range(22):
                    MM(py[sp_][:, :n], wd[:, c, m * 128:(m + 1) * 128], gab[s][:, c, :n], c == 0, c == 21, [b_wd, b_gab[s]], [b_py[sp_]])
                STT(xab[s][:, m, :n], py[sp_][:, :n], MR(0, j, "gf", m), xab[s][:, m, :n], ALU.mult, ALU.add,
                    [b_py[sp_], b_msc, b_xab[s]], [b_xab[s]])
            ln_stats([xab[s][:, m, :n] for m in range(8)], n, 1024, st, b_st, ps1, ps2, b_ps1, b_ps2, [b_xab[s]])

        def tail(ci):
            t0, n = chunks[ci]; s = ci % 2
            j = 1 if t0 >= NE else 0
            resid_ln_tail(xab[s], b_xab[s], n, st, b_st, xc, b_xc, xab[s], oh[s], b_xab[s], b_oh[s],
                          (f"A2s0{j}", f"A2b0{j}", f"H2s0{j}", f"H2b0{j}"))
            P.dma('sp', xav2[:, :, t0:t0 + n], xab[s][:, :, :n], reads=[b_xab[s]])
            P.dma('sp', hv2[:, :, t0:t0 + n], oh[s][:, :, :n], reads=[b_oh[s]])

        loads(0); loads(1)
        mm_stats(0)
        for ci in range(NCH):
            tail(ci)
            if ci + 1 < NCH:
                mm_stats(ci + 1)
            if ci + 2 < NCH:
                loads(ci + 2)

    def phaseF(es, post_init=None):
        NB = 4; L = 3
        hb = sb(es, "hb", [128, 8, NT0], BF16)
        wq = [sb(es, f"wq{s}", [128, 3, 8, 128], BF16) for s in range(2)]
        qT = [sb(es, f"qT{s}", [128, NOWN], BF16) for s in range(2)]
        kTz = [[sb(es, f"kT{s}{h}", [128, NT0], BF16) for h in range(2)] for s in range(2)]
        va = [sb(es, f"va{s}", [128, 22, 256], BF16) for s in range(2)]
        tb = sb(es, "tb", [128, 2, 14 * 64])
        tI = [sb(es, f"tI{s}", [128, 2, 14 * 64]) for s in range(2)]
        tA = [sb(es, f"tA{s}", [128, 2, 14 * 64]) for s in range(2)]
        msk = sb(es, "msk", [128, 2, 14 * 64])
        vrow = sb(es, "vrow", [128, 48])
        ex = [sb(es, f"ex{s}", [128, 512]) for s in range(NB)]
        pT = [sb(es, f"pT{s}", [128, 512], BF16) for s in range(NB)]
        rs = [sb(es, f"rs{s}", [128, 256]) for s in range(2)]
        ob = [sb(es, f"ob{s}", [128, NOWN], BF16) for s in range(2)]
        pq = [psum(es, f"pq{s}", [128, 512]) for s in range(2)]
        pss = [psum(es, f"pss{s}", [128, 512]) for s in range(NB)]
        po = [psum(es, f"po{s}", [128, 512]) for s in range(2)]
        b_wq = P.bufs(2); b_ex = P.bufs(NB); b_pT = P.bufs(NB); b_rs = P.bufs(2); b_pq = P.bufs(2); b_pss = P.bufs(NB); b_po = P.bufs(2)
        b_qT = P.bufs(2); b_kT = P.bufs(2); b_va = P.bufs(2); b_tab = P.bufs(2); b_ob = P.bufs(2)
        b_hb, b_tb, b_msk = P.bufs(3)
        P.dma('sp', hb[:], h2_s.rearrange("p (c t) -> p c t", c=8), writes=[b_hb])
        P.dma('sp', msk[:], namask.rearrange("p (a f) -> p a f", a=2), writes=[b_msk])
        P.dma('sp', vrow[:], navrow, writes=[b_msk])
        for s in range(2):
            MEMSET('pool', va[s][:], 1.0, [b_va[s]])
            MEMSET('dve', kTz[s][0][64:128, :], 0.0, [b_kT[s]])
            MEMSET('dve', kTz[s][1][0:64, :], 0.0, [b_kT[s]])
        o1v = o1_s.rearrange("p (c t) -> p c t", c=8)
        cnt = {'pq': 0}

        def prologue(pr):
            s = pr % 2
            P.dma_group('pool', [(wq[s][:, a, :, :], w_qkv.rearrange("(k p) c -> p k c", p=128)[:, :, a * D + pr * 128:a * D + (pr + 1) * 128])
                                 for a in range(3)], writes=[b_wq[s]])
            P.dma_group('sp', [(tb[:, hh, :], nabias[pr * 2 + hh]) for hh in range(2)], writes=[b_tb])
            ACT(tb[:], tb[:], AF.Exp, [b_tb], [b_tb])
            TT('pool', tI[s][:], tb[:], bc_mid(msk[:, 0, :], 2), ALU.mult, [b_tb, b_msk], [b_tab[s]])
            TT('pool', tA[s][:], tb[:], bc_mid(msk[:, 1, :], 2), ALU.mult, [b_tb, b_msk], [b_tab[s]])
            for ci in range(4):
                sp_ = cnt['pq'] % 2; cnt['pq'] += 1
                for k in range(8):
                    MM(pq[sp_][:, :], wq[s][:, 0, k, :], hb[:, k, 256 + ci * 512:256 + (ci + 1) * 512], k == 0, k == 7, [b_wq[s], b_hb], [b_pq[sp_]])
                ACT(qT[s][:, ci * 512:(ci + 1) * 512], pq[sp_][:, :], AF.Copy, [b_pq[sp_]], [b_qT[s]], scale=0.125)
            for ci in range(6):
                n = 512 if ci < 5 else 256
                sp_ = cnt['pq'] % 2; cnt['pq'] += 1
                for k in range(8):
                    MM(pq[sp_][:, :n], wq[s][:, 1, k, :], hb[:, k, ci * 512:ci * 512 + n], k == 0, k == 7, [b_wq[s], b_hb], [b_pq[sp_]])
                CP('dve', kTz[s][0][0:64, ci * 512:ci * 512 + n], pq[sp_][0:64, :n], [b_pq[sp_]], [b_kT[s]])
                CP('act', kTz[s][1][64:128, ci * 512:ci * 512 + n], pq[sp_][64:128, :n], [b_pq[sp_]], [b_kT[s]])
            for t4 in range(0, 22, 4):
                nt = min(4, 22 - t4)
                sp_ = cnt['pq'] % 2; cnt['pq'] += 1
                for ti in range(nt):
                    t = t4 + ti
                    for k in range(8):
                        MM(pq[sp_][:, ti * 128:(ti + 1) * 128], hb[:, k, t * 128:(t + 1) * 128], wq[s][:, 2, k, :], k == 0, k == 7, [b_hb, b_wq[s]], [b_pq[sp_]])
                pv4 = pq[sp_][:, :nt * 128].rearrange("p (t h d) -> p t h d", h=2, d=64)
                CP('act', va[s][:, t4:t4 + nt, 0:64], pv4[:, :, 0, :], [b_pq[sp_]], [b_va[s]])
                CP('act', va[s][:, t4:t4 + nt, 192:256], pv4[:, :, 1, :], [b_pq[sp_]], [b_va[s]])

        items = [(pr, g, j) for pr in range(8) for g in range(8) for j in range(8)]

        def key_of(g, j):
            return (2 * g + j, j) if j < 6 else (20 + (j - 6), None)

        def qk_exp(i):
            pr, g, j = items[i]
            if g == 0 and j == 0:
                prologue(pr)
                if pr == 1 and post_init:
                    post_init()
            s = pr % 2; s3 = i % NB; q0 = g * 256
            kt, ch = key_of(g, j)
            special = g in (0, 7)
            for hh in range(2):
                MM(pss[s3][:, hh * 256:(hh + 1) * 256], kTz[s][hh][:, kt * 128:(kt + 1) * 128], qT[s][:, q0:q0 + 256], True, True,
                   [b_kT[s], b_qT[s]], [b_pss[s3]])
            if ch is None:
                ACT(pT[s3][:, :], pss[s3][:, :], AF.Exp, [b_pss[s3]], [b_pT[s3]])
            else:
                ACT(ex[s3][:, :], pss[s3][:, :], AF.Exp, [b_pss[s3]], [b_ex[s3]])
                sl = 10 - 2 * ch
                exv = ex[s3][:, :].rearrange("p (h q) -> p h q", h=2)
                pTv = pT[s3][:, :].rearrange("p (h q) -> p h q", h=2)
                if special:
                    TT('dve', exv, exv, tA[s][:, :, sl * 64:(sl + 4) * 64], ALU.mult, [b_ex[s3], b_tab[s]], [b_ex[s3]])
                    vi = (0 if g == 0 else 1) * 24 + ch * 4
                    va_ = vrow[:, vi:vi + 4]
                    vb = bass.AP(tensor=va_.tensor, offset=va_.offset, ap=[list(va_.ap[0]), [0, 2], [1, 4], [0, 64]])
                    TT('dve', pT[s3][:, :].rearrange("p (h i q) -> p h i q", h=2, i=4), ex[s3][:, :].rearrange("p (h i q) -> p h i q", h=2, i=4),
                       vb, ALU.mult, [b_ex[s3], b_msk], [b_pT[s3]])
                else:
                    TT('dve', pTv, exv, tI[s][:, :, sl * 64:(sl + 4) * 64], ALU.mult, [b_ex[s3], b_tab[s]], [b_pT[s3]])

        def pv(i):
            pr, g, j = items[i]
            s = pr % 2; s3 = i % NB; q0 = g * 256
            kt, ch = key_of(g, j)
            for hh in range(2):
                lo = 0 if hh == 0 else 128
                MM(po[hh][:, :256], va[s][:, kt, lo:lo + 128], pT[s3][:, hh * 256:(hh + 1) * 256], j == 0, j == 7, [b_va[s], b_pT[s3]], [b_po[hh]])
            if j == 7:
                for hh in range(2):
                    ob_, sm_ = (0, 64) if hh == 0 else (64, 0)
                    ACT(rs[hh][ob_:ob_ + 64, :], po[hh][sm_:sm_ + 64, :256], AF.Ln, [b_po[hh]], [b_rs[hh]])
                    ACT(rs[hh][ob_:ob_ + 64, :], rs[hh][ob_:ob_ + 64, :], AF.Exp, [b_rs[hh]], [b_rs[hh]], scale=-1.0)
                    TT('dve', ob[s][ob_:ob_ + 64, q0:q0 + 256], po[hh][ob_:ob_ + 64, :256], rs[hh][ob_:ob_ + 64, :], ALU.mult,
                       [b_po[hh], b_rs[hh]], [b_ob[s]])
                if g == 7:
                    P.dma('sp', o1v[:, pr, :], ob[s][:], reads=[b_ob[s]])

        for i in range(len(items) + L):
            if i < len(items):
                qk_exp(i)
            if i >= L:
                pv(i - L)

    def phaseG(es, pre=None):
        wo = pre['wo'][0] if pre else sb(es, "wo", [128, 8, D], BF16)
        wr = pre['wr'][0] if pre else sb(es, "wr", [128, 8, 8], BF16)
        ao = [sb(es, f"ao{s}", [128, 8, 512], BF16) for s in range(2)]
        xa = [sb(es, f"xa{s}", [128, 8, 512]) for s in range(2)]
        vt = [sb(es, f"vt{s}", [128, 8, 512]) for s in range(2)]
        oxa = [sb(es, f"oxa{s}", [128, 8, 512]) for s in range(2)]
        oh = [sb(es, f"oh{s}", [128, 8, 512], BF16) for s in range(2)]
        xc = sb(es, "xcb", [128, 8, 512])
        xtm = [sb(es, f"xtm{s}", [128, D]) for s in range(2)]
        lg = sb(es, "lg", [128, 8]); mx = sb(es, "mx", [128, 8]); eq = sb(es, "eq", [128, 8]); wts = sb(es, "wts", [128, 4])
        st, b_st = mk_st(es, "g", LN_EPS)
        ptr = psum(es, "ptr", [128, 1024]); py = [psum(es, f"py{s}", [128, 512]) for s in range(2)]
        ps1 = psum(es, "ps1", [128, 512]); ps2 = psum(es, "ps2", [128, 512]); plg = psum(es, "plg", [128, 512])
        b_ao = P.bufs(2); b_xa = P.bufs(2); b_vt = P.bufs(2); b_oxa = P.bufs(2); b_oh = P.bufs(2); b_xc = P.buf(); b_py = P.bufs(2); b_xtm = P.bufs(2)
        b_w, b_ptr, b_ps1, b_ps2, b_plg, b_lg = P.bufs(6)
        if pre:
            b_w = pre['wo'][1]
        else:
            P.dma_group('pool', [(wo[:], w_out1.rearrange("(k p) c -> p k c", p=128)),
                                 (wr[:], w_router.rearrange("(k p) c -> p k c", p=128))], writes=[b_w])
        o1v = o1_s.rearrange("p (c t) -> p c t", c=8)
        xav = xa2_s.rearrange("p (c t) -> p c t", c=8)
        hv = h3_s.rearrange("p (c t) -> p c t", c=8)
        tix = 0

        def prep(ci):
            s = ci % 2; n = 512; t0 = ci * 512
            P.dma('sp', ao[s][:], o1v[:, :, t0:t0 + n], writes=[b_ao[s]])
            P.dma('sp', xa[s][:], xav[:, :, 256 + t0:256 + t0 + n], writes=[b_xa[s]])

        tixc = {'i': 0}

        def mm_stats(ci):
            s = ci % 2; n = 512
            for m in range(8):
                sp_ = m % 2
                for k in range(8):
                    MM(py[sp_][:, :n], wo[:, k, m * 128:(m + 1) * 128], ao[s][:, k, :n], k == 0, k == 7, [b_w, b_ao[s]], [b_py[sp_]])
                STT(vt[s][:, m, :n], py[sp_][:, :n], MR(1, 0, "gm", m), xa[s][:, m, :n], ALU.mult, ALU.add,
                    [b_py[sp_], b_msc, b_xa[s]], [b_vt[s]])
            ln_stats([vt[s][:, m, :n] for m in range(8)], n, 1024, st, b_st, ps1, ps2, b_ps1, b_ps2, [b_vt[s]])

        def tail(ci):
            s = ci % 2; n = 512; t0 = ci * 512
            resid_ln_tail(vt[s], b_vt[s], n, st, b_st, xc, b_xc, oxa[s], oh[s], b_oxa[s], b_oh[s],
                          ("A1s10", "A1b10", "H1s10", "H1b10"))
            P.dma('sp', hv[:, :, t0:t0 + n], oh[s][:], reads=[b_oh[s]])

        def post(ci):
            s = ci % 2
            for ti in range(4):
                s2 = tixc['i'] % 2; tixc['i'] += 1
                tile_i = ci * 4 + ti
                for k in range(8):
                    TR(ptr[:, k * 128:(k + 1) * 128], oxa[s][:, k, ti * 128:(ti + 1) * 128], identf[:], [b_oxa[s], b_const], [b_ptr])
                CP('act', xtm[s2][:], ptr[:], [b_ptr], [b_xtm[s2]])
                P.dma('sp', xa3_s[tile_i * 128:(tile_i + 1) * 128, :], xtm[s2][:], reads=[b_xtm[s2]])
                for k in range(8):
                    MM(plg[:, 0:8], oh[s][:, k, ti * 128:(ti + 1) * 128], wr[:, k, :], k == 0, k == 7, [b_oh[s], b_w], [b_plg])
                CP('dve', lg[:], plg[:, 0:8], [b_plg], [b_lg])
                P.op('dve', lambda e: e.max(out=mx[:], in_=lg[:]), [b_lg], [b_lg])
                TT('dve', wts[:, 0:1], mx[:, 0:1], mx[:, 1:2], ALU.subtract, [b_lg], [b_lg])
                TT('dve', wts[:, 1:2], mx[:, 1:2], mx[:, 0:1], ALU.subtract, [b_lg], [b_lg])
                ACT(wts[:, 2:4], wts[:, 0:2], AF.Sigmoid, [b_lg], [b_lg])
                gt = gates[:, tile_i * 8:(tile_i + 1) * 8]
                TS('dve', eq[:], lg[:], mx[:, 0:1], wts[:, 2:3], ALU.is_equal, ALU.mult, [b_lg], [b_lg])
                TS('dve', gt, lg[:], mx[:, 1:2], wts[:, 3:4], ALU.is_equal, ALU.mult, [b_lg], [b_gates])
                TT('dve', gt, gt, eq[:], ALU.add, [b_lg, b_gates], [b_gates])

        prep(0); prep(1)
        mm_stats(0)
        for ci in range(4):
            tail(ci)
            if ci + 1 < 4:
                mm_stats(ci + 1)
            post(ci)
            if ci + 2 < 4:
                prep(ci + 2)

    def phaseH(es):
        hb = sb(es, "hb", [128, 8, NOWN], BF16)
        yacc = sb(es, "yacc", [128, 16, D])
        wgu = [sb(es, f"wgu{s}", [128, 2, 8, 512], BF16) for s in range(2)]
        wdn = [sb(es, f"wdn{s}", [128, 4, D], BF16) for s in range(2)]
        sg = [sb(es, f"sg{s}", [128, 512]) for s in range(2)]
        ga = [sb(es, f"ga{s}", [128, 4, 512], BF16) for s in range(2)]
        pg = [psum(es, f"pg{s}", [128, 512]) for s in range(2)]; pu = [psum(es, f"pu{s}", [128, 512]) for s in range(2)]
        py = [psum(es, f"py{s}", [128, 512]) for s in range(4)]
        b_wgu = P.bufs(2); b_wdn = P.bufs(2); b_sg = P.bufs(2); b_ga = P.bufs(2); b_pg = P.bufs(2); b_pu = P.bufs(2); b_py = P.bufs(4)
        b_hb, = P.bufs(1)
        b_y = P.bufs(16)
        P.dma('sp', hb[:], h3_s.rearrange("p (c t) -> p c t", c=8), writes=[b_hb])
        cnts = {'isg': 0, 'ipy': 0}
        steps = [(e, cg, tc) for e in range(NEXP) for cg in range(7) for tc in range(4)]

        def gate_up(i):
            e, cg, tc = steps[i]
            gi = e * 7 + cg
            s = gi % 2
            c0 = cg * 4
            if tc == 0:
                P.dma_group('pool', [(wgu[s][:, 0, :, :], mwg[e].rearrange("(k p) c -> p k c", p=128)[:, :, c0 * 128:(c0 + 4) * 128]),
                                     (wgu[s][:, 1, :, :], mwu[e].rearrange("(k p) c -> p k c", p=128)[:, :, c0 * 128:(c0 + 4) * 128])],
                            writes=[b_wgu[s]])
                P.dma('pool', wdn[s][:], mwd[e].rearrange("(c p) m -> p c m", p=128)[:, c0:c0 + 4, :], writes=[b_wdn[s]])
            o = tc * 512
            sga = i % 2
            for cc in range(4):
                sp_ = cnts['isg'] % 2; cnts['isg'] += 1
                for k in range(8):
                    MM(pg[sp_][:, :], wgu[s][:, 0, k, cc * 128:(cc + 1) * 128], hb[:, k, o:o + 512], k == 0, k == 7, [b_wgu[s], b_hb], [b_pg[sp_]])
                for k in range(8):
                    MM(pu[sp_][:, :], wgu[s][:, 1, k, cc * 128:(cc + 1) * 128], hb[:, k, o:o + 512], k == 0, k == 7, [b_wgu[s], b_hb], [b_pu[sp_]])
                ACT(sg[sp_][:, :], pg[sp_][:, :], AF.Silu, [b_pg[sp_]], [b_sg[sp_]])
                TT('dve', ga[sga][:, cc, :], pu[sp_][:, :], sg[sp_][:, :], ALU.mult, [b_pu[sp_], b_sg[sp_]], [b_ga[sga]])

        def down(i):
            e, cg, tc = steps[i]
            gi = e * 7 + cg
            s = gi % 2
            sga = i % 2
            first = (e == 0 and cg == 0)
            for ti in range(4):
                tile_i = tc * 4 + ti
                for hf in range(2):
                    sy = cnts['ipy'] % 4; cnts['ipy'] += 1
                    for cc in range(4):
                        MM(py[sy][:, :], ga[sga][:, cc, ti * 128:(ti + 1) * 128], wdn[s][:, cc, hf * 512:(hf + 1) * 512], cc == 0, cc == 3,
                           [b_ga[sga], b_wdn[s]], [b_py[sy]])
                    ya = yacc[:, tile_i, hf * 512:(hf + 1) * 512]
                    gsc = gates[:, tile_i * 8 + e:tile_i * 8 + e + 1]
                    if first:
                        TS('dve', ya, py[sy][:, :], gsc, None, ALU.mult, None, [b_py[sy], b_gates], [b_y[tile_i]])
                    else:
                        STT(ya, py[sy][:, :], gsc, ya, ALU.mult, ALU.add, [b_py[sy], b_gates, b_y[tile_i]], [b_y[tile_i]])

        for i in range(len(steps) + 1):
            if i < len(steps):
                gate_up(i)
            if i >= 1:
                down(i - 1)
        for t in range(16):
            P.dma('sp', y_s[t * 128:(t + 1) * 128, :], yacc[:, t, :], reads=[b_y[t]])

    def phaseI(es):
        lnr = sb(es, "lnr", [128, 2 * D])
        gfr = sb(es, "gfr", [128, D])
        scb = sb(es, "scb", [128, 8, 2], BF16)
        wmr = sb(es, "wmr", [128, 8, D], BF16)
        row = sb(es, "row", [1, D]); brow = sb(es, "brow", [1, D])
        xr = [sb(es, f"xr{s}", [128, D]) for s in range(3)]
        yr = [sb(es, f"yr{s}", [128, D]) for s in range(3)]
        vv = [sb(es, f"vv{s}", [128, D]) for s in range(3)]
        stt3 = [sb(es, f"stt{q}", [128, 2, 6]) for q in range(3)]; ag3 = [sb(es, f"ag{q}", [128, 2]) for q in range(3)]
        rstd3 = [sb(es, f"rstd{q}", [128, 1]) for q in range(3)]; nmr3 = [sb(es, f"nmr{q}", [128, 1]) for q in range(3)]
        epsb = sb(es, "epsb", [128, 1]); b_st3 = P.bufs(3)
        csb = sb(es, "csb", [128, 16])
        pg = [psum(es, "pgi", [128, 512])]; pu = [psum(es, "pui", [128, 512])]
        b_pg = P.bufs(1); b_pu = P.bufs(1)
        b_xr = P.bufs(3); b_vv = P.bufs(3); b_yr = P.bufs(3)
        b_lnr, b_gfr, b_row, b_st = P.bufs(4)
        P.dma('sp', lnr[:], lnrow, writes=[b_lnr])
        MEMSET('dve', epsb[:], LN_EPS, [b_st])
        P.dma('sp', csb[:], cT, writes=[b_row])
        ACT(scb[:].rearrange("p k j -> p (k j)"), csb[:], AF.Silu, [b_row], [b_row])
        P.dma('pool', wmr[:], w_mod[1].rearrange("(k p) c -> p k c", p=128)[:, :, 5 * D:6 * D], writes=[b_row])
        P.dma('sp', brow[:], bmodrow, writes=[b_row])
        for hf in range(2):
            for k in range(8):
                MM(pg[0][0:2, :], scb[:, k, :], wmr[:, k, hf * 512:(hf + 1) * 512], k == 0, k == 7, [b_row], [b_pg[0]])
            TT('dve', row[0:1, hf * 512:(hf + 1) * 512], pg[0][0:1, :], brow[0:1, hf * 512:(hf + 1) * 512], ALU.add, [b_pg[0], b_row], [b_row])
        for hf in range(2):
            MM(pu[0][:, :], onesf[0:1, :], row[0:1, hf * 512:(hf + 1) * 512], True, True, [b_row, b_const], [b_pu[0]])
            CP('dve', gfr[:, hf * 512:(hf + 1) * 512], pu[0][:, :], [b_pu[0]], [b_gfr])
        def prep(t):
            s = t % 3
            P.dma('sp', xr[s][:], xa3_s[t * 128:(t + 1) * 128, :], writes=[b_xr[s]])
            P.dma('sp', yr[s][:], y_s[t * 128:(t + 1) * 128, :], writes=[b_yr[s]])

        prep(0); prep(1)
        for t in range(16):
            s = t % 3
            if t + 2 < 16:
                prep(t + 2)
            TT('dve', vv[s][:], yr[s][:], gfr[:], ALU.mult, [b_yr[s], b_gfr], [b_vv[s]])
            TT('dve', vv[s][:], vv[s][:], xr[s][:], ALU.add, [b_vv[s], b_xr[s]], [b_vv[s]])
            stt, ag, rstd, nmr, b_sq = stt3[s], ag3[s], rstd3[s], nmr3[s], b_st3[s]
            for hf in range(2):
                P.op('dve', lambda e, o_=stt[:, hf, :], i_=vv[s][:, hf * 512:(hf + 1) * 512]: e.bn_stats(out=o_, in_=i_), [b_vv[s]], [b_sq])
            P.op('dve', lambda e, o_=ag[:], i_=stt[:].rearrange("p a b -> p (a b)"): e.bn_aggr(out=o_, in_=i_), [b_sq], [b_sq])
            ACT(rstd[:], ag[:, 1:2], AF.Sqrt, [b_sq, b_st], [b_sq], bias=epsb[:, 0:1])
            RECIP(rstd[:], rstd[:], [b_sq], [b_sq])
            TS('dve', nmr[:], ag[:, 0:1], -1.0, None, ALU.mult, None, [b_sq], [b_sq])
            TT('dve', nmr[:], nmr[:], rstd[:], ALU.mult, [b_sq], [b_sq])
            ACT(vv[s][:], vv[s][:], AF.Identity, [b_vv[s], b_sq], [b_vv[s]], scale=rstd[:, 0:1], bias=nmr[:, 0:1])
            TT('dve', vv[s][:], vv[s][:], lnr[:, 0:D], ALU.mult, [b_vv[s], b_lnr], [b_vv[s]])
            TT('dve', vv[s][:], vv[s][:], lnr[:, D:2 * D], ALU.add, [b_vv[s], b_lnr], [b_vv[s]])
            P.dma('sp', out_d[t * 128:(t + 1) * 128, :], vv[s][:], reads=[b_vv[s]])

    def run(fn, **kw):
        with contextlib.ExitStack() as es:
            fn(es, **kw)
            P.flush()

    ES = contextlib.ExitStack
    run(phase0)
    with ES() as o:
        wA = sb(o, "wA_pre", [128, 8, 1536], BF16); b_wA = P.buf()
        run(phaseA, post_init=lambda: P.dma('pool', wA[:], w_in.rearrange("(k p) c -> p k c", p=128)[:, :, 0:1536], writes=[b_wA]))
        run(phaseB, pre={'wA': (wA, b_wA)})
    with ES() as o:
        wo0 = sb(o, "wo0_pre", [128, 8, D], BF16); b_wo0 = P.buf()
        run(phaseD, post_init=lambda: P.dma('pool', wo0[:], w_out0.rearrange("(k p) c -> p k c", p=128), writes=[b_wo0]))
        run(phaseE1, pre={'wo': (wo0, b_wo0)})
    with ES() as o:
        wdp = sb(o, "wd_pre", [128, 22, D], BF16); b_wdp = P.buf()
        run(phaseE2a, post_init=lambda: P.dma_group('pool', [(wdp[:, 0:11, :], wd0.rearrange("(c p) m -> p c m", p=128)[:, 0:11, :]),
                                                              (wdp[:, 11:22, :], wd0.rearrange("(c p) m -> p c m", p=128)[:, 11:22, :])], writes=[b_wdp]))
        run(phaseE2b, pre={'wd': (wdp, b_wdp)})
    with ES() as o:
        wo1 = sb(o, "wo1_pre", [128, 8, D], BF16); wr1 = sb(o, "wr1_pre", [128, 8, 8], BF16); b_wo1 = P.buf()
        run(phaseF, post_init=lambda: P.dma_group('pool', [(wo1[:], w_out1.rearrange("(k p) c -> p k c", p=128)),
                                                            (wr1[:], w_router.rearrange("(k p) c -> p k c", p=128))], writes=[b_wo1]))
        run(phaseG, pre={'wo': (wo1, b_wo1), 'wr': (wr1, b_wo1)})
    run(phaseH)
    run(phaseI)
    P.close()
    return nc


def _rope_tables(pos_r, pos_c, scale):
    half = 16
    inv = 10000.0 ** (-np.arange(half, dtype=np.float32) * 2.0 / 32)
    ar = pos_r.astype(np.float32)[:, None] * inv[None, :]
    ac = pos_c.astype(np.float32)[:, None] * inv[None, :]
    cr, sr, cc, sc = np.cos(ar), np.sin(ar), np.cos(ac), np.sin(ac)
    cos = np.concatenate([cr, cr, cc, cc], axis=1)
    sin = np.concatenate([-sr, sr, -sc, sc], axis=1)
    return (np.concatenate([cos, sin], axis=1) * scale).astype(np.float32)


def _core_inputs(b, half, inp):
    x = inp['x'][b]; ctx = inp['ctx'][b]
    own = half * 2048
    d = {}
    idx2 = own - 384 + np.arange(NE2)
    ok2 = (idx2 >= 0) & (idx2 < NALL)
    xe2 = np.zeros((NE2, D), np.float32); xe2[ok2] = x[idx2[ok2]]
    d['xe2'] = xe2
    d['xall'] = np.ascontiguousarray(x)
    c2 = np.zeros((NC2, D), np.float32); c2[128:128 + NC] = ctx
    d['ctx2'] = c2
    mc = np.zeros(NC2, np.float32); mc[128:128 + NC] = 1
    d['me2'] = np.ascontiguousarray(np.broadcast_to(np.concatenate([ok2.astype(np.float32), mc])[None, :], (128, NE2 + NC2)))
    idxe = own - 256 + np.arange(NE)
    oke = (idxe >= 0) & (idxe < NALL)
    pe = np.where(oke, idxe, 0)
    rq = _rope_tables(pe // 64, pe % 64, 0.125)
    ident = np.concatenate([np.ones((NC, 64), np.float32), np.zeros((NC, 64), np.float32)], axis=1)
    d['ropeq'] = np.concatenate([rq, ident * 0.125], axis=0)
    pa = np.arange(NALL)
    d['ropek'] = np.concatenate([ident, _rope_tables(pa // 64, pa % 64, 1.0)], axis=0)
    cT = np.zeros((128, 8, 2), np.float32)
    cT[:, :, 0] = inp['c'][b].reshape(8, 128).T; cT[:, :, 1] = inp['c_ctx'].reshape(8, 128).T
    d['cT'] = cT.reshape(128, 16)
    d['bmodT'] = np.ascontiguousarray(inp['b_mod'].reshape(2, 48, 128).transpose(2, 0, 1).reshape(128, 96))
    ln = np.stack([inp['ln_g'], inp['ln_b']], axis=2)
    d['lnT'] = np.ascontiguousarray(ln.reshape(2, 2, 2, 8, 128).transpose(4, 0, 1, 2, 3).reshape(128, 64))
    d['convwT'] = np.ascontiguousarray(inp['ab_conv_w'][0].reshape(31, 4, 128).transpose(2, 1, 0).reshape(128, 124))
    cgb = np.stack([inp['ab_conv_g'][0], inp['ab_conv_b'][0]], axis=0)
    d['convgbT'] = np.ascontiguousarray(cgb.reshape(2, 4, 128).transpose(2, 0, 1).reshape(128, 8))
    d['qkg'] = np.ascontiguousarray(np.broadcast_to(np.concatenate([inp['ab_q_g'][0], inp['ab_k_g'][0]])[None, :], (128, 128)))
    d['identf'] = np.eye(128, dtype=np.float32)
    for k in ('w_mod',):
        d[k] = inp[k]
    d['ab_w_in'] = inp['ab_w_in'][0]; d['ab_w_out'] = inp['ab_w_out'][0]
    d['ffn_w_gate'] = inp['ffn_w_gate'][0]; d['ffn_w_up'] = inp['ffn_w_up'][0]; d['ffn_w_down'] = inp['ffn_w_down'][0]
    d['na_w_qkv'] = inp['na_w_qkv'][0]; d['na_w_out'] = inp['na_w_out'][0]
    d['moe_w_router'] = inp['moe_w_router'][0]
    d['moe_w_gate'] = inp['moe_w_gate'][0]; d['moe_w_up'] = inp['moe_w_up'][0]; d['moe_w_down'] = inp['moe_w_down'][0]
    rpb = inp['na_rpb'][0]
    kc = np.arange(64)[:, None]; qc = np.arange(64)[None, :]
    dc = np.clip(kc - qc + 15, 0, 30)
    cstart = np.clip(np.arange(64) - 8, 0, 48)[None, :]
    colok = ((kc >= cstart) & (kc < cstart + 16)).astype(np.float32)
    nb = np.zeros((16, 128, 14, 64), np.float32)
    mI = np.zeros((128, 14, 64), np.float32); mA = np.zeros((128, 14, 64), np.float32)
    for hf in range(2):
        for sp in range(14):
            dr = (13 - sp) + hf
            nb[:, hf * 64:(hf + 1) * 64, sp, :] = rpb[:, dr][:, dc]
            mA[hf * 64:(hf + 1) * 64, sp, :] = colok
            if 3 <= dr <= 10:
                mI[hf * 64:(hf + 1) * 64, sp, :] = colok
    d['nabias'] = nb.reshape(16, 128, 14 * 64)
    d['namask'] = np.concatenate([mI.reshape(128, -1), mA.reshape(128, -1)], axis=1)
    vr = np.zeros((128, 2, 6, 4), np.float32)
    for which, g in enumerate((0, 7)):
        for ch in range(6):
            for i in range(4):
                qr = half * 32 + 4 * g + i
                r0 = min(max(qr - 4, 0), 56)
                for hf in range(2):
                    kr = half * 32 + 4 * g - 4 + 2 * ch + hf
                    if r0 <= kr < r0 + 8:
                        vr[hf * 64:(hf + 1) * 64, which, ch, i] = 1
    d['navrow'] = vr.reshape(128, 48)
    lr = np.concatenate([inp['ln_g'][1, 1], inp['ln_b'][1, 1]])
    d['lnrow'] = np.ascontiguousarray(np.broadcast_to(lr[None, :], (128, 2 * D)))
    d['bmodrow'] = np.ascontiguousarray(inp['b_mod'][1, 5 * D:6 * D][None, :])
    return d


_NC_CACHE = {}


def kernel(**inputs):
    inp = {k: np.asarray(v, dtype=np.float32) for k, v in inputs.items()}
    if 'nc' not in _NC_CACHE:
        _NC_CACHE['nc'] = build_program()
    nc = _NC_CACHE['nc']
    in_maps = [_core_inputs(r // 2, r % 2, inp) for r in range(8)]
    res = run_bass_kernel_spmd(nc, in_maps, core_ids=list(range(8)))
    out = np.zeros((4, 4096, D), np.float32)
    for r in range(8):
        b, half = r // 2, r % 2
        out[b, half * 2048:(half + 1) * 2048] = res.results[r]["out"]
    return out
```

```python
import contextlib
import numpy as np
import concourse.bass as bass
import concourse.mybir as mybir
from concourse.bass_utils import run_bass_kernel_spmd

F32 = mybir.dt.float32; BF16 = mybir.dt.bfloat16
AF = mybir.ActivationFunctionType
ALU = mybir.AluOpType
AX = mybir.AxisListType

D = 1024
NE = 2560; NE2 = 2816; NC = 256; NC2 = 512; NALL = 4096; NKEY = 4352
NT0 = NE + NC
NOWN = 2048
FFN = 2816; EXP = 3584; NEXP = 8
ALPHA = 4 ** 0.25
LN_EPS = 1e-5; RMS_EPS = 1e-6


class Buf:
    __slots__ = ("name", "w", "readers")
    def __init__(self, name):
        self.name = name; self.w = {}; self.readers = {}

EPOCH = 30000
NDMASEM = 10


class Prog:
    def __init__(self, nc):
        self.nc = nc
        self.es = contextlib.ExitStack()
        self.engs = ['sp', 'pe', 'act', 'dve', 'pool']
        self.streams = {k: [] for k in self.engs}
        self.cnt = {k: 0 for k in self.engs}
        self.seen = {k: {} for k in self.engs}
        self.sems = {}
        self.dma_pool = {}
        self.dma_rr = {}
        for q in ('sp', 'pool'):
            self.dma_pool[q] = [[self._sem(f"d_{q}_{i}"), 0, f"d_{q}_{i}"] for i in range(NDMASEM)]
            self.dma_rr[q] = 0
        self.nbuf = 0

    def _sem(self, name):
        s = self.es.enter_context(self.nc.semaphore(name))
        self.sems[name] = s
        return s

    def buf(self, name=None):
        self.nbuf += 1
        return Buf(name or f"b{self.nbuf}")

    def bufs(self, n, name="b"):
        return [self.buf(f"{name}{i}") for i in range(n)]

    def _eng_sem(self, eng, epoch):
        key = f"e_{eng}_{epoch}"
        if key not in self.sems:
            self._sem(key)
        return key

    def _wait(self, eng, deps):
        for key, val in deps.items():
            if self.seen[eng].get(key, 0) >= val:
                continue
            self.seen[eng][key] = val
            sem = self.sems[key]
            self.streams[eng].append(lambda e, sem=sem, val=val: e.wait_ge(sem, val))

    def _collect(self, eng, reads, writes):
        deps = {}
        def add(tok):
            if tok is None:
                return
            k, v = tok
            if eng == 'pe' and k.startswith('e_pe_'):
                return
            if deps.get(k, 0) < v:
                deps[k] = v
        for b in reads:
            for k, v in b.w.items():
                add((k, v))
        for b in writes:
            for k, v in b.w.items():
                add((k, v))
            for k, v in b.readers.items():
                add((k, v))
        return deps

    def _commit(self, tok, reads, writes, toks=None):
        toks = toks or [tok]
        for b in writes:
            b.w = {}; b.readers = {}
            for k, v in toks:
                if b.w.get(k, 0) < v:
                    b.w[k] = v
        ws = set(id(b) for b in writes)
        for b in reads:
            if id(b) in ws:
                continue
            for k, v in toks:
                if b.readers.get(k, 0) < v:
                    b.readers[k] = v

    def op(self, eng, fn, reads=(), writes=()):
        deps = self._collect(eng, reads, writes)
        self._wait(eng, deps)
        n = self.cnt[eng]; self.cnt[eng] = n + 1
        epoch, idx = divmod(n, EPOCH)
        key = self._eng_sem(eng, epoch)
        sem = self.sems[key]
        self.streams[eng].append(lambda e, fn=fn, sem=sem: fn(e).then_inc(sem, 1))
        tok = (key, idx + 1)
        self._commit(tok, reads, writes)
        return tok

    def dma(self, q, out, in_, reads=(), writes=()):
        pool = self.dma_pool[q]
        i = self.dma_rr[q]; self.dma_rr[q] = (i + 1) % len(pool)
        ent = pool[i]
        deps = self._collect(q, reads, writes)
        if ent[1] > 0 and deps.get(ent[2], 0) < ent[1]:
            deps[ent[2]] = ent[1]
        self._wait(q, deps)
        ent[1] += 16
        sem = ent[0]
        self.streams[q].append(lambda e, out=out, in_=in_, sem=sem: e.dma_start(out=out, in_=in_).then_inc(sem, 16))
        tok = (ent[2], ent[1])
        self._commit(tok, reads, writes)
        return tok

    def dma_group(self, q, pairs, reads=(), writes=()):
        base = self._collect(q, reads, writes)
        toks = []
        pool = self.dma_pool[q]
        for (out, in_) in pairs:
            i = self.dma_rr[q]; self.dma_rr[q] = (i + 1) % len(pool)
            ent = pool[i]
            deps = dict(base)
            if ent[1] > 0 and deps.get(ent[2], 0) < ent[1]:
                deps[ent[2]] = ent[1]
            self._wait(q, deps)
            ent[1] += 16
            sem = ent[0]
            self.streams[q].append(lambda e, out=out, in_=in_, sem=sem: e.dma_start(out=out, in_=in_).then_inc(sem, 16))
            toks.append((ent[2], ent[1]))
        self._commit(None, reads, writes, toks=toks)
        return toks

    def barrier(self):
        deps = {}
        for q, pool in self.dma_pool.items():
            for ent in pool:
                if ent[1] > 0:
                    deps[ent[2]] = ent[1]
        for e in self.engs:
            n = self.cnt[e]
            if n > 0:
                epoch, idx = divmod(n - 1, EPOCH)
                deps[self._eng_sem(e, epoch)] = idx + 1
        for e in self.engs:
            d = {k: v for k, v in deps.items() if not k.startswith(f"e_{e}_")}
            self._wait(e, d)

    def flush(self):
        self.barrier()
        streams = self.streams
        self.streams = {k: [] for k in self.engs}
        with self.nc.Block() as block:
            def mk(name):
                def f(e):
                    for c in streams[name]:
                        c(e)
                return f
            block.sync(mk('sp'))
            block.tensor(mk('pe'))
            block.scalar(mk('act'))
            block.vector(mk('dve'))
            block.gpsimd(mk('pool'))

    def close(self):
        self.es.close()


def build_program(stop_after=None, debug=False):
    nc = bass.Bass("TRN2", target_bir_lowering=False)
    P = Prog(nc)

    def dram_in(name, shape, dt=F32):
        return nc.dram_tensor(name, list(shape), dt, kind="ExternalInput").ap()

    def dram_scr(name, shape, dt=F32):
        if debug:
            return nc.dram_tensor(name, list(shape), dt, kind="ExternalOutput").ap()
        return nc.dram_tensor(name, list(shape), dt).ap()

    xe2 = dram_in("xe2", [NE2, D]); xall = dram_in("xall", [NALL, D]); ctx2 = dram_in("ctx2", [NC2, D])
    me2 = dram_in("me2", [128, NE2 + NC2])
    ropeq = dram_in("ropeq", [NT0, 128]); ropek = dram_in("ropek", [NKEY, 128])
    cT = dram_in("cT", [128, 16]); bmodT = dram_in("bmodT", [128, 96]); lnT = dram_in("lnT", [128, 64])
    convwT = dram_in("convwT", [128, 4 * 31]); convgbT = dram_in("convgbT", [128, 8])
    qkg = dram_in("qkg", [128, 128]); identf_d = dram_in("identf", [128, 128])
    w_mod = dram_in("w_mod", [2, D, 6 * D])
    w_in = dram_in("ab_w_in", [D, 1792]); w_out0 = dram_in("ab_w_out", [D, D])
    wg0 = dram_in("ffn_w_gate", [D, FFN]); wu0 = dram_in("ffn_w_up", [D, FFN]); wd0 = dram_in("ffn_w_down", [FFN, D])
    w_qkv = dram_in("na_w_qkv", [D, 3 * D]); w_out1 = dram_in("na_w_out", [D, D])
    nabias = dram_in("nabias", [16, 128, 14 * 64]); namask = dram_in("namask", [128, 14 * 64 * 2])
    navrow = dram_in("navrow", [128, 2 * 6 * 4])
    w_router = dram_in("moe_w_router", [D, NEXP])
    mwg = dram_in("moe_w_gate", [NEXP, D, EXP]); mwu = dram_in("moe_w_up", [NEXP, D, EXP]); mwd = dram_in("moe_w_down", [NEXP, EXP, D])
    lnrow = dram_in("lnrow", [128, 2 * D]); bmodrow = dram_in("bmodrow", [1, D])
    out_d = nc.dram_tensor("out", [NOWN, D], F32, kind="ExternalOutput").ap()

    kT_s = dram_scr("kT_s", [128, 2 * NKEY], BF16)
    va_s = dram_scr("va_s", [128, 34 * 2 * 192], BF16)
    qT_s = dram_scr("qT_s", [128, 4 * NT0], BF16)
    u_s = dram_scr("u_s", [128, 4 * (NE2 + NC2)], F32)
    ao_s = dram_scr("ao_s", [128, 8 * NT0], BF16)
    xa1_s = dram_scr("xa1_s", [128, 8 * NT0], F32)
    h1_s = dram_scr("h1_s", [128, 8 * NT0], BF16)
    xa2_s = dram_scr("xa2_s", [128, 8 * NT0], F32)
    h2_s = dram_scr("h2_s", [128, 8 * NT0], BF16)
    o1_s = dram_scr("o1_s", [128, 8 * NOWN], BF16)
    h3_s = dram_scr("h3_s", [128, 8 * NOWN], BF16)
    xa3_s = dram_scr("xa3_s", [NOWN, D], F32)
    y_s = dram_scr("y_s", [NOWN, D], F32)
    msc_s = dram_scr("msc_s", [128, 1024], F32)
    ga_s = dram_scr("ga_s", [128, 22 * NT0], BF16)
    cv_s = dram_scr("cv_s", [128, 4 * NT0], F32)
    dbg = {}

    pes = P.es
    shared = {}

    _uid = [0]

    def sb(es, name, shape, dt=F32):
        _uid[0] += 1
        return es.enter_context(nc.sbuf_tensor(f"s{_uid[0]}_{name}", list(shape), dt))

    def psum(es, name, shape, dt=F32):
        _uid[0] += 1
        return es.enter_context(nc.psum_tensor(f"p{_uid[0]}_{name}", list(shape), dt))

    def MM(out, lhsT, rhs, start, stop, reads, writes):
        return P.op('pe', lambda e: e.matmul(out, lhsT, rhs, start=start, stop=stop), reads, writes)

    def TR(out, in_, ident, reads, writes):
        return P.op('pe', lambda e: e.transpose(out, in_, ident), reads, writes)

    def ACT(out, in_, func, reads, writes, scale=None, bias=None):
        kw = {}
        if scale is not None: kw['scale'] = scale
        if bias is not None: kw['bias'] = bias
        return P.op('act', lambda e: e.activation(out=out, in_=in_, func=func, **kw), reads, writes)

    def TT(eng, out, in0, in1, op, reads, writes):
        return P.op(eng, lambda e: e.tensor_tensor(out=out, in0=in0, in1=in1, op=op), reads, writes)

    def TS(eng, out, in0, s1, s2, op0, op1, reads, writes):
        if op1 is None:
            return P.op(eng, lambda e: e.tensor_scalar(out=out, in0=in0, scalar1=s1, scalar2=None, op0=op0), reads, writes)
        return P.op(eng, lambda e: e.tensor_scalar(out=out, in0=in0, scalar1=s1, scalar2=s2, op0=op0, op1=op1), reads, writes)

    def STT(out, in0, scalar, in1, op0, op1, reads, writes):
        return P.op('dve', lambda e: e.scalar_tensor_tensor(out=out, in0=in0, scalar=scalar, in1=in1, op0=op0, op1=op1), reads, writes)

    def CP(eng, out, in_, reads, writes):
        if eng == 'act':
            return P.op(eng, lambda e: e.activation(out=out, in_=in_, func=AF.Copy), reads, writes)
        return P.op(eng, lambda e: e.tensor_copy(out=out, in_=in_), reads, writes)

    def RECIP(out, in_, reads, writes):
        return P.op('dve', lambda e: e.reciprocal(out=out, in_=in_), reads, writes)

    def MEMSET(eng, out, val, writes):
        return P.op(eng, lambda e: e.memset(out, val), (), writes)

    def bc_free(ap2d, n_outer, n_inner_bcast):
        a = ap2d.ap
        return bass.AP(tensor=ap2d.tensor, offset=ap2d.offset, ap=[list(a[0]), list(a[1]), [0, n_inner_bcast]])

    def bc_mid(ap2d, n_mid):
        a = ap2d.ap
        return bass.AP(tensor=ap2d.tensor, offset=ap2d.offset, ap=[list(a[0]), [0, n_mid], list(a[1])])

    identf = sb(pes, "identf", [128, 128]); identb = sb(pes, "identb", [128, 128], BF16)
    onesf = sb(pes, "onesf", [128, 128]); onesb = sb(pes, "onesb", [128, 128], BF16)
    msc = sb(pes, "msc", [128, 1024])
    gates = sb(pes, "gates", [128, 16 * 8])
    b_const = P.buf("const"); b_msc = P.buf("msc"); b_gates = P.buf("gates")
    P.dma('sp', identf[:], identf_d, writes=[b_const])
    P.dma('pool', identb[:], identf_d, writes=[b_const])
    MEMSET('dve', onesf[:], 1.0, [b_const])
    MEMSET('dve', onesb[:], 1.0, [b_const])

    cols = {}
    _c = [0]
    def mcol(name, n=8):
        cols[name] = _c[0]; _c[0] += n
        return cols[name]
    for i in range(2):
        for j in range(2):
            mcol(f"raw{i}{j}", 48)
    for i in range(2):
        for j in range(2):
            for nm in ("opscm", "opscf", "A1s", "A1b", "H1s", "H1b", "A2s", "A2b", "H2s", "H2b"):
                mcol(f"{nm}{i}{j}")
    mcol("ln", 64); mcol("convw", 124); mcol("convgb", 8); mcol("bmod", 96)
    def M(name, k=0, n=1):
        c = cols[name] + k
        return msc[:, c:c + n]
    RAW = {"shm": 0, "scm": 8, "gm": 16, "shf": 24, "scf": 32, "gf": 40}
    def MR(i, j, what, k=0, n=1):
        return M(f"raw{i}{j}", RAW[what] + k, n)
    def LN(i, j, gb, k=0, n=1):
        return M("ln", ((i * 2 + j) * 2 + gb) * 8 + k, n)

    def phase0(es):
        csb = sb(es, "csb", [128, 16]); scT = sb(es, "scT", [128, 16], BF16)
        wm = [sb(es, f"wm{s}", [128, 8, 1536], BF16) for s in range(2)]
        pmod = psum(es, "pmod", [128, 512])
        b_c, b_sc, b_pm = P.bufs(3); b_wm = P.bufs(2)
        P.dma('sp', csb[:], cT, writes=[b_c])
        P.dma('sp', M("ln", 0, 64), lnT, writes=[b_msc])
        P.dma('sp', M("convw", 0, 124), convwT, writes=[b_msc])
        P.dma('sp', M("convgb", 0, 8), convgbT, writes=[b_msc])
        P.dma('sp', M("bmod", 0, 96), bmodT, writes=[b_msc])
        ACT(scT[:], csb[:], AF.Silu, [b_c], [b_sc])
        it = 0
        for i in range(2):
            for grp in range(4):
                s = it % 2; it += 1
                P.dma('pool', wm[s][:], w_mod[i].rearrange("(k p) c -> p k c", p=128)[:, :, grp * 1536:(grp + 1) * 1536], writes=[b_wm[s]])
                for mm in range(12):
                    m = grp * 12 + mm
                    for k in range(8):
                        MM(pmod[:, (i * 48 + m) * 2:(i * 48 + m) * 2 + 2], wm[s][:, k, mm * 128:(mm + 1) * 128],
                           scT[:, 2 * k:2 * k + 2], k == 0, k == 7, [b_wm[s], b_sc], [b_pm])
        pv = pmod[:, 0:192].rearrange("p (i m j) -> p i m j", i=2, j=2)
        for i in range(2):
            for j in range(2):
                TT('dve', M(f"raw{i}{j}", 0, 48), pv[:, i, :, j], M("bmod", i * 48, 48), ALU.add, [b_pm, b_msc], [b_msc])
        for i in range(2):
            for j in range(2):
                TS('dve', M(f"opscm{i}{j}", 0, 8), MR(i, j, "scm", 0, 8), 1.0, None, ALU.add, None, [b_msc], [b_msc])
                TS('dve', M(f"opscf{i}{j}", 0, 8), MR(i, j, "scf", 0, 8), 1.0, None, ALU.add, None, [b_msc], [b_msc])
        for i in range(2):
            for j in range(2):
                TS('dve', M(f"A1s{i}{j}", 0, 8), LN(i, 0, 0, 0, 8), ALPHA, None, ALU.mult, None, [b_msc], [b_msc])
                TS('dve', M(f"A1b{i}{j}", 0, 8), LN(i, 0, 1, 0, 8), ALPHA, None, ALU.mult, None, [b_msc], [b_msc])
                TT('dve', M(f"H1s{i}{j}", 0, 8), LN(i, 0, 0, 0, 8), M(f"opscf{i}{j}", 0, 8), ALU.mult, [b_msc], [b_msc])
                TT('dve', M(f"H1b{i}{j}", 0, 8), LN(i, 0, 1, 0, 8), M(f"opscf{i}{j}", 0, 8), ALU.mult, [b_msc], [b_msc])
                TT('dve', M(f"H1b{i}{j}", 0, 8), M(f"H1b{i}{j}", 0, 8), MR(i, j, "shf", 0, 8), ALU.add, [b_msc], [b_msc])
        for j in range(2):
            TS('dve', M(f"A2s0{j}", 0, 8), LN(0, 1, 0, 0, 8), ALPHA, None, ALU.mult, None, [b_msc], [b_msc])
            TS('dve', M(f"A2b0{j}", 0, 8), LN(0, 1, 1, 0, 8), ALPHA, None, ALU.mult, None, [b_msc], [b_msc])
            TT('dve', M(f"H2s0{j}", 0, 8), LN(0, 1, 0, 0, 8), M(f"opscm1{j}", 0, 8), ALU.mult, [b_msc], [b_msc])
            TT('dve', M(f"H2b0{j}", 0, 8), LN(0, 1, 1, 0, 8), M(f"opscm1{j}", 0, 8), ALU.mult, [b_msc], [b_msc])
            TT('dve', M(f"H2b0{j}", 0, 8), M(f"H2b0{j}", 0, 8), MR(1, j, "shm", 0, 8), ALU.add, [b_msc], [b_msc])
        if debug:
            P.dma('sp', msc_s, msc[:], reads=[b_msc])

    def load_T(src_rows, xt, b_xt, ptr, b_ptr):
        P.dma('sp', xt[:], src_rows, writes=[b_xt])
        for k in range(8):
            TR(ptr[:, k * 128:(k + 1) * 128], xt[:, k * 128:(k + 1) * 128], identf[:], [b_xt, b_const], [b_ptr])

    def rms_rope(src, nh, gain_ap, rope_t, tmp, b_tmp, reads, dst, dst_writes, dup=False):
        n = nh * 64
        B = b_tmp
        srcv = src.rearrange("p (h d) -> p h d", d=64)
        ACT(tmp['sq'][:, :n], src, AF.Square, reads, [B['sq']])
        P.op('dve', lambda e: e.tensor_reduce(out=tmp['ss'][:, :nh], in_=tmp['sq'][:, :n].rearrange("p (h d) -> p h d", d=64),
                                              axis=AX.X, op=ALU.add), [B['sq']], [B['ss']])
        TS('dve', tmp['ss'][:, :nh], tmp['ss'][:, :nh], 1.0 / 64, RMS_EPS, ALU.mult, ALU.add, [B['ss']], [B['ss']])
        ACT(tmp['rs'][:, :nh], tmp['ss'][:, :nh], AF.Sqrt, [B['ss']], [B['ss']])
        RECIP(tmp['rs'][:, :nh], tmp['rs'][:, :nh], [B['ss']], [B['ss']])
        knv = tmp['kn'][:, :n].rearrange("p (h d) -> p h d", d=64)
        TT('dve', knv, srcv, bc_free(tmp['rs'][:, :nh], nh, 64), ALU.mult, reads + [B['ss']], [B['kn']])
        TT('dve', knv, knv, bc_mid(gain_ap, nh), ALU.mult, [B['kn'], b_const], [B['kn']])
        kn5 = tmp['kn'][:, :n].rearrange("p (h g s d) -> p h g s d", g=2, s=2, d=16)
        t25 = tmp['t2'][:, :n].rearrange("p (h g s d) -> p h g s d", g=2, s=2, d=16)
        sin4 = rope_t[:, 64:128].rearrange("p (g s d) -> p g s d", g=2, s=2, d=16)
        for s in range(2):
            for g in range(2):
                sa = sin4[:, g, s, :]
                sinb = bass.AP(tensor=sa.tensor, offset=sa.offset, ap=[list(sa.ap[0]), [0, nh], list(sa.ap[1])])
                TT('pool', t25[:, :, g, s, :], kn5[:, :, g, 1 - s, :], sinb, ALU.mult, [B['kn']] + reads, [B['t2']])
        t1v = tmp['t1'][:, :n].rearrange("p (h d) -> p h d", d=64)
        TT('dve', t1v, knv, bc_mid(rope_t[:, 0:64], nh), ALU.mult, [B['kn']] + reads, [B['t1']])
        t2v = tmp['t2'][:, :n].rearrange("p (h d) -> p h d", d=64)
        if dup:
            for dd in range(2):
                TT('dve', dst[:, :, dd, :], t1v, t2v, ALU.add, [B['t1'], B['t2']], dst_writes)
        else:
            TT('dve', dst, t1v, t2v, ALU.add, [B['t1'], B['t2']], dst_writes)

    def mk_tmp(es, pfx):
        t = {'sq': sb(es, pfx + "sq", [128, 512]), 'ss': sb(es, pfx + "ss", [128, 8]), 'rs': sb(es, pfx + "rs", [128, 8]),
             'kn': sb(es, pfx + "kn", [128, 512]), 't1': sb(es, pfx + "t1", [128, 512]), 't2': sb(es, pfx + "t2", [128, 512])}
        b = {k: P.buf() for k in ('sq', 'ss', 'kn', 't1', 't2')}
        return t, b

    def phaseA(es, post_init=None):
        xt = [sb(es, f"xt{s}", [128, D]) for s in range(2)]
        hT = [sb(es, f"hT{s}", [128, 8, 128], BF16) for s in range(2)]
        rk = [sb(es, f"rk{s}", [128, 128]) for s in range(2)]
        wkv = sb(es, "wkv", [128, 8, 256], BF16)
        qkg_t = sb(es, "qkg_t", [128, 128])
        kdup = [sb(es, f"kdup{s}", [128, 256], BF16) for s in range(2)]
        kT = sb(es, "kT", [128, 2 * NKEY], BF16)
        va = sb(es, "va", [128, 34 * 2 * 192], BF16)
        tmps = [mk_tmp(es, f"a{q}") for q in range(2)]
        ptr = [psum(es, f"ptr{s}", [128, 1024]) for s in range(2)]
        pkv = [psum(es, f"pkv{s}", [128, 512]) for s in range(2)]
        pkt = [psum(es, f"pkt{s}", [128, 256], BF16) for s in range(2)]
        b_xt = P.bufs(2); b_hT = P.bufs(2); b_rk = P.bufs(2); b_kd = P.bufs(2); b_ptr = P.bufs(2); b_pkv = P.bufs(2); b_pkt = P.bufs(2)
        b_w, b_kT, b_va = P.bufs(3)
        P.dma('pool', wkv[:], w_in.rearrange("(k p) c -> p k c", p=128)[:, :, 1536:1792], writes=[b_w])
        if post_init:
            post_init()
        P.dma('sp', qkg_t[:], qkg, writes=[b_const])
        MEMSET('pool', va[:], 1.0, [b_va])
        kTv = kT[:].rearrange("p (g t) -> p g t", g=2)
        vav = va[:].rearrange("p (t g c) -> p t g c", g=2, c=192)
        def st1(t):
            s = t % 2
            j = 1 if t < 2 else 0
            src = ctx2[128 + 128 * t:256 + 128 * t, :] if t < 2 else xall[(t - 2) * 128:(t - 1) * 128, :]
            load_T(src, xt[s], b_xt[s], ptr[s], b_ptr[s])
            P.dma('sp', rk[s][:], ropek[t * 128:(t + 1) * 128, :], writes=[b_rk[s]])
            for k in range(8):
                ACT(hT[s][:, k, :], ptr[s][:, k * 128:(k + 1) * 128], AF.Identity, [b_ptr[s], b_msc], [b_hT[s]],
                    scale=M(f"opscm0{j}", k), bias=MR(0, j, "shm", k))

        def st2(t):
            s = t % 2
            for k in range(8):
                MM(pkv[s][:, 0:256], hT[s][:, k, :], wkv[:, k, :], k == 0, k == 7, [b_hT[s], b_w], [b_pkv[s]])
            kd4 = kdup[s][:].rearrange("p (g dd d) -> p g dd d", g=2, dd=2)
            rms_rope(pkv[s][:, 0:128], 2, qkg_t[:, 64:128], rk[s], tmps[s][0], tmps[s][1], [b_pkv[s], b_rk[s]], kd4, [b_kd[s]], dup=True)
            for off in (0, 128):
                CP('dve', vav[:, t, :, off:off + 64], pkv[s][:, 128:256].rearrange("p (g d) -> p g d", g=2),
                   [b_pkv[s], tmps[s][1]['kn']], [b_va])

        def st3(t):
            s = t % 2
            for g in range(2):
                TR(pkt[s][:, g * 128:(g + 1) * 128], kdup[s][:, g * 128:(g + 1) * 128], identb[:], [b_kd[s], b_const], [b_pkt[s]])
            CP('act', kTv[:, :, t * 128:(t + 1) * 128], pkt[s][:].rearrange("p (g t) -> p g t", g=2), [b_pkt[s]], [b_kT])

        for t in range(34 + 2):
            if t < 34:
                st1(t)
            if 1 <= t <= 34:
                st2(t - 1)
            if t >= 2:
                st3(t - 2)
        P.dma('sp', kT_s, kT[:], reads=[b_kT])
        P.dma('sp', va_s, va[:], reads=[b_va])

    def phaseB(es, pre=None):
        xt = [sb(es, f"xt{s}", [128, D]) for s in range(2)]
        hT = [sb(es, f"hT{s}", [128, 8, 512], BF16) for s in range(2)]
        rq = [sb(es, f"rq{s}", [128, 128]) for s in range(8)]
        wA = pre['wA'][0] if pre else sb(es, "wA", [128, 8, 1536], BF16)
        qkg_t = sb(es, "qkg_t", [128, 128])
        mk = [sb(es, f"mk{s}", [128, 512]) for s in range(2)]
        sg = [sb(es, f"sg{s}", [128, 512]) for s in range(2)]
        ut = [sb(es, f"ut{s}", [128, 4, 512]) for s in range(2)]
        qr = [sb(es, f"qr{s}", [128, 512], BF16) for s in range(2)]
        qTt = [sb(es, f"qTt{s}", [128, 4, 128], BF16) for s in range(2)]
        tmps = [mk_tmp(es, f"b{q}") for q in range(2)]
        ptr = psum(es, "ptr", [128, 1024])
        pa = [psum(es, f"pa{s}", [128, 512]) for s in range(4)]
        pq = psum(es, "pq", [128, 512])
        pqt = psum(es, "pqt", [128, 512], BF16)
        b_xt = P.bufs(2); b_hT = P.bufs(2); b_rq = P.bufs(8); b_mk = P.bufs(2); b_sg = P.bufs(2); b_ut = P.bufs(2)
        b_qr = P.bufs(2); b_qTt = P.bufs(2); b_pa = P.bufs(4)
        b_w, b_ptr, b_pq, b_pqt = P.bufs(4)
        if pre:
            b_w = pre['wA'][1]
        else:
            P.dma('pool', wA[:], w_in.rearrange("(k p) c -> p k c", p=128)[:, :, 0:1536], writes=[b_w])
        P.dma('sp', qkg_t[:], qkg, writes=[b_const])
        uv = u_s.rearrange("p (c t) -> p c t", c=4)
        qv = qT_s.rearrange("p (c t) -> p c t", c=4)
        chunks = []
        for ci in range(6):
            n = 512 if ci < 5 else 256
            chunks.append((0, xe2, ci * 512, n, ci * 512))
        chunks.append((1, ctx2, 0, 512, NE2))
        tcnt = {'i': 0}

        def qtile_t0(j, grow):
            if j == 0:
                if grow < 128 or grow >= 128 + NE:
                    return None
                return grow - 128
            if grow < 128 or grow >= 128 + NC:
                return None
            return NE + (grow - 128)

        pend = []

        def q_finish(t0, s3):
            for pr in range(4):
                TR(pqt[:, pr * 128:(pr + 1) * 128], qr[s3][:, pr * 128:(pr + 1) * 128], identb[:], [b_qr[s3], b_const], [b_pqt])
            CP('act', qTt[s3][:], pqt[:].rearrange("p (c t) -> p c t", c=4), [b_pqt], [b_qTt[s3]])
            P.dma('sp', qv[:, :, t0:t0 + 128], qTt[s3][:], reads=[b_qTt[s3]])

        def prep(cidx):
            (j, src, r0, n, ucol) = chunks[cidx]
            s = cidx % 2
            nt = n // 128
            P.dma('sp', mk[s][:, :n], me2[:, ucol:ucol + n], writes=[b_mk[s]])
            for ti in range(nt):
                t0_ = qtile_t0(j, r0 + ti * 128)
                if t0_ is not None:
                    sr = (t0_ // 128) % 8
                    P.dma('sp', rq[sr][:], ropeq[t0_:t0_ + 128, :], writes=[b_rq[sr]])
            for ti in range(nt):
                s2 = tcnt['i'] % 2; tcnt['i'] += 1
                load_T(src[r0 + ti * 128:r0 + (ti + 1) * 128, :], xt[s2], b_xt[s2], ptr, b_ptr)
                for k in range(8):
                    ACT(hT[s][:, k, ti * 128:(ti + 1) * 128], ptr[:, k * 128:(k + 1) * 128], AF.Identity, [b_ptr, b_msc], [b_hT[s]],
                        scale=M(f"opscm0{j}", k), bias=MR(0, j, "shm", k))

        prep(0)
        for cidx, (j, src, r0, n, ucol) in enumerate(chunks):
            s = cidx % 2
            nt = n // 128
            if cidx + 1 < len(chunks):
                prep(cidx + 1)
            def do_q(ti):
                t0 = qtile_t0(j, r0 + ti * 128)
                s3 = (t0 // 128) % 2
                sr = (t0 // 128) % 8
                for k in range(8):
                    MM(pq[:, :], hT[s][:, k, ti * 128:(ti + 1) * 128], wA[:, k, 1024:1536], k == 0, k == 7, [b_hT[s], b_w], [b_pq])
                rms_rope(pq[:, :], 8, qkg_t[:, 0:64], rq[sr], tmps[s3][0], tmps[s3][1], [b_pq, b_rq[sr]],
                         qr[s3][:].rearrange("p (h d) -> p h d", d=64), [b_qr[s3]])
                if pend:
                    q_finish(*pend.pop())
                pend.append((t0, s3))

            qtiles = [ti for ti in range(nt) if qtile_t0(j, r0 + ti * 128) is not None]
            for c in range(4):
                if c < len(qtiles):
                    do_q(qtiles[c])
                pv_, pg_ = pa[(2 * c) % 4], pa[(2 * c + 1) % 4]
                bv_, bg_ = b_pa[(2 * c) % 4], b_pa[(2 * c + 1) % 4]
                for k in range(8):
                    MM(pv_[:, :n], wA[:, k, c * 128:(c + 1) * 128], hT[s][:, k, :n], k == 0, k == 7, [b_w, b_hT[s]], [bv_])
                for k in range(8):
                    MM(pg_[:, :n], wA[:, k, 512 + c * 128:512 + (c + 1) * 128], hT[s][:, k, :n], k == 0, k == 7, [b_w, b_hT[s]], [bg_])
                ss_ = c % 2
                ACT(sg[ss_][:, :n], pg_[:, :n], AF.Sigmoid, [bg_], [b_sg[ss_]])
                TT('pool', sg[ss_][:, :n], sg[ss_][:, :n], mk[s][:, :n], ALU.mult, [b_sg[ss_], b_mk[s]], [b_sg[ss_]])
                TT('dve', ut[s][:, c, :n], pv_[:, :n], sg[ss_][:, :n], ALU.mult, [bv_, b_sg[ss_]], [b_ut[s]])
            P.dma('sp', uv[:, :, ucol:ucol + n], ut[s][:, :, :n], reads=[b_ut[s]])
        while pend:
            q_finish(*pend.pop())

    def ln_stats(vchunks, n, nfeat, st, b_st, ps1, ps2, b_ps1, b_ps2, reads):
        nch = len(vchunks)
        for c, v in enumerate(vchunks):
            MM(ps1[:, :n], onesf[:], v, c == 0, c == nch - 1, reads + [b_const], [b_ps1])
        for c, v in enumerate(vchunks):
            q = c % 2
            ACT(st['sqb'][q][:, :n], v, AF.Square, reads, [b_st['sq'][q]])
            MM(ps2[:, :n], onesb[:], st['sqb'][q][:, :n], c == 0, c == nch - 1, [b_st['sq'][q], b_const], [b_ps2])
        ACT(st['mean'][:, :n], ps1[:, :n], AF.Copy, [b_ps1], [b_st['m']], scale=1.0 / nfeat)
        TT('dve', st['msq'][:, :n], st['mean'][:, :n], st['mean'][:, :n], ALU.mult, [b_st['m']], [b_st['r']])
        STT(st['rstd'][:, :n], ps2[:, :n], 1.0 / nfeat, st['msq'][:, :n], ALU.mult, ALU.subtract, [b_ps2, b_st['r']], [b_st['r']])
        TS('dve', st['rstd'][:, :n], st['rstd'][:, :n], 0.0, None, ALU.max, None, [b_st['r']], [b_st['r']])
        ACT(st['rstd'][:, :n], st['rstd'][:, :n], AF.Sqrt, [b_st['r']], [b_st['r']], bias=st['eps'][:, 0:1])
        RECIP(st['rstd'][:, :n], st['rstd'][:, :n], [b_st['r']], [b_st['r']])

    def mk_st(es, pfx, eps):
        st = {'sqb': [sb(es, pfx + f"sqb{q}", [128, 512], BF16) for q in range(2)], 'mean': sb(es, pfx + "mean", [128, 512]),
              'msq': sb(es, pfx + "msq", [128, 512]), 'rstd': sb(es, pfx + "rstd", [128, 512]), 'eps': sb(es, pfx + "eps", [128, 1])}
        b_st = {'sq': P.bufs(2), 'm': P.buf(), 'r': P.buf()}
        MEMSET('dve', st['eps'][:], eps, [b_st['r']])
        return st, b_st

    def phaseC_gen(es):
        ub = [sb(es, f"ub{s}", [128, 4, 512 + 32]) for s in range(2)]
        acc = [sb(es, f"acc{s}", [128, 4, 512]) for s in range(2)]
        b_ub = P.bufs(2); b_acc = [P.bufs(4) for _ in range(2)]
        uv = u_s.rearrange("p (c t) -> p c t", c=4)
        cvv = cv_s.rearrange("p (c t) -> p c t", c=4)
        chunks = [(128 + ci * 512, 512, ci * 512) for ci in range(5)] + [(NE2 + 128, 256, NE)]
        for ci, (ucol, n, t0) in enumerate(chunks):
            s = ci % 2
            P.dma('pool', ub[s][:, :, :n + 32], uv[:, :, ucol - 16:ucol + n + 16], writes=[b_ub[s]])
            yield
            for k in range(31):
                for c in range(4):
                    src = ub[s][:, c, 1 + k:1 + k + n]
                    w = M("convw", c * 31 + k)
                    if k == 0:
                        TS('dve', acc[s][:, c, :n], src, w, None, ALU.mult, None, [b_ub[s], b_msc], [b_acc[s][c]])
                    else:
                        STT(acc[s][:, c, :n], src, w, acc[s][:, c, :n], ALU.mult, ALU.add, [b_ub[s], b_msc, b_acc[s][c]], [b_acc[s][c]])
                    yield
            P.dma('pool', cvv[:, :, t0:t0 + n], acc[s][:, :, :n], reads=list(b_acc[s]))
            yield

    def phaseD(es, post_init=None):
        NB = 3; L = 2
        cgen = phaseC_gen(es)
        kTz = [sb(es, f"kTz{h}", [128, 2 * NKEY], BF16) for h in range(2)]
        va = sb(es, "va", [128, 34 * 2 * 192], BF16)
        qT = [sb(es, f"qT{s}", [128, 4, 512], BF16) for s in range(2)]
        pT = [sb(es, f"pT{s}", [128, 2, 512], BF16) for s in range(NB)]
        rs = [sb(es, f"rs{s}", [128, 512]) for s in range(2)]
        ob = [sb(es, f"ob{s}", [128, 4, 512], BF16) for s in range(2)]
        pss = [psum(es, f"pss{s}", [128, 1024]) for s in range(NB)]
        po = [psum(es, f"po{s}", [128, 512]) for s in range(2)]
        b_qT = P.bufs(2); b_pT = P.bufs(NB); b_rs = P.bufs(2); b_ob = P.bufs(2); b_pss = P.bufs(NB); b_po = P.bufs(2)
        b_kT, b_va = P.bufs(2)
        P.dma_group('sp', [(kTz[0][:], kT_s), (kTz[1][:], kT_s)], writes=[b_kT])
        P.dma('sp', va[:], va_s, writes=[b_va])
        if post_init:
            post_init()
        MEMSET('dve', kTz[0][64:128, :], 0.0, [b_kT])
        MEMSET('pool', kTz[1][0:64, :], 0.0, [b_kT])
        kTv = [kTz[h][:].rearrange("p (g t) -> p g t", g=2) for h in range(2)]
        vav = va[:].rearrange("p (t g c) -> p t g c", g=2, c=192)
        qv = qT_s.rearrange("p (c t) -> p c t", c=4)
        aov = ao_s.rearrange("p (c t) -> p c t", c=8)
        chunks = [(ci * 512, 512, 34) for ci in range(5)] + [(NE, 256, 2)]
        items = []
        for ci, (t0, n, nkc) in enumerate(chunks):
            for pr in range(4):
                for kc in range(nkc):
                    items.append((ci, t0, n, nkc, pr, kc))

        def qk_exp(i):
            ci, t0, n, nkc, pr, kc = items[i]
            s = ci % 2; g = pr // 2; s3 = i % NB
            if pr == 0 and kc == 0:
                P.dma('sp', qT[s][:, :, :n], qv[:, :, t0:t0 + n], writes=[b_qT[s]])
            for hh in range(2):
                MM(pss[s3][:, hh * 512:hh * 512 + n], kTv[hh][:, g, kc * 128:(kc + 1) * 128], qT[s][:, pr, :n], True, True,
                   [b_kT, b_qT[s]], [b_pss[s3]])
            ACT(pT[s3][:, :, :n], pss[s3][:].rearrange("p (h q) -> p h q", h=2)[:, :, :n], AF.Exp, [b_pss[s3]], [b_pT[s3]])

        def pv(i):
            ci, t0, n, nkc, pr, kc = items[i]
            s = ci % 2; g = pr // 2; s3 = i % NB
            for hh in range(2):
                lo = 0 if hh == 0 else 64
                MM(po[hh][:, :n], vav[:, kc, g, lo:lo + 128], pT[s3][:, hh, :n], kc == 0, kc == nkc - 1, [b_va, b_pT[s3]], [b_po[hh]])
            if kc == nkc - 1:
                for hh in range(2):
                    ob_, sm_ = (0, 64) if hh == 0 else (64, 0)
                    RECIP(rs[hh][ob_:ob_ + 64, :n], po[hh][sm_:sm_ + 64, :n], [b_po[hh]], [b_rs[hh]])
                    TT('dve', ob[s][ob_:ob_ + 64, pr, :n], po[hh][ob_:ob_ + 64, :n], rs[hh][ob_:ob_ + 64, :n], ALU.mult,
                       [b_po[hh], b_rs[hh]], [b_ob[s]])
                if pr == 3:
                    P.dma('sp', aov[:, 4:8, t0:t0 + n], ob[s][:, :, :n], reads=[b_ob[s]])

        for i in range(len(items) + L):
            if i < len(items):
                qk_exp(i)
            if i >= L:
                pv(i - L)
            for _ in range(2 if i % 4 == 0 else 1):
                next(cgen, None)
        for _ in cgen:
            pass

    def resid_ln(vt, b_vt, n, st, b_st, ps1, ps2, b_ps1, b_ps2, xc, b_xc, out_xa, out_h, b_oxa, b_oh, names, o=0):
        ln_stats([vt[:, m, o:o + n] for m in range(8)], n, 1024, st, b_st, ps1, ps2, b_ps1, b_ps2, [b_vt])
        resid_ln_tail(vt, b_vt, n, st, b_st, xc, b_xc, out_xa, out_h, b_oxa, b_oh, names, o)

    def resid_ln_tail(vt, b_vt, n, st, b_st, xc, b_xc, out_xa, out_h, b_oxa, b_oh, names, o=0):
        As, Ab, Hs, Hb = names
        TT('dve', xc[:, :, :n], vt[:, :, o:o + n], bc_mid(st['mean'][:, :n], 8), ALU.subtract, [b_vt, b_st['m']], [b_xc])
        TT('dve', xc[:, :, :n], xc[:, :, :n], bc_mid(st['rstd'][:, :n], 8), ALU.mult, [b_xc, b_st['r']], [b_xc])
        for m in range(8):
            ACT(out_xa[:, m, :n], xc[:, m, :n], AF.Identity, [b_xc, b_msc], [b_oxa], scale=M(As, m), bias=M(Ab, m))
            ACT(out_h[:, m, :n], xc[:, m, :n], AF.Identity, [b_xc, b_msc], [b_oh], scale=M(Hs, m), bias=M(Hb, m))

    def phaseE1(es, pre=None):
        xt = [sb(es, f"xt{s}", [128, D]) for s in range(4)]
        wo = pre['wo'][0] if pre else sb(es, "wo", [128, 8, D], BF16)
        ao = [sb(es, f"ao{s}", [128, 8, 512], BF16) for s in range(2)]
        xa = [sb(es, f"xa{s}", [128, 8, 512]) for s in range(2)]
        vt = [sb(es, f"vt{s}", [128, 8, 512]) for s in range(2)]
        oxa = [sb(es, "oxa0", [128, 8, 512])] * 2
        oh = [sb(es, f"oh{s}", [128, 8, 512], BF16) for s in range(2)]
        xc = sb(es, "xcb", [128, 8, 512])
        st, b_st = mk_st(es, "e", LN_EPS)
        cvt = [sb(es, f"cvt{s}", [128, 4, 512]) for s in range(2)]
        xc2 = xc[:, 0:4, :]
        st2, b_st2 = mk_st(es, "e2", LN_EPS)
        b_cvt = P.bufs(2)
        cvv = cv_s.rearrange("p (c t) -> p c t", c=4)
        ptr = psum(es, "ptr", [128, 1024]); py = [psum(es, f"py{s}", [128, 512]) for s in range(2)]
        ps1 = psum(es, "ps1", [128, 512]); ps2 = psum(es, "ps2", [128, 512])
        b_xt = P.bufs(4); b_ao = P.bufs(2); b_xa = P.bufs(2); b_vt = P.bufs(2); b_oxa = [P.buf()] * 2; b_oh = P.bufs(2); b_xc = P.buf(); b_py = P.bufs(2)
        b_w, b_ptr, b_ps1, b_ps2 = P.bufs(4)
        if pre:
            b_w = pre['wo'][1]
        else:
            P.dma('pool', wo[:], w_out0.rearrange("(k p) c -> p k c", p=128), writes=[b_w])
        aov = ao_s.rearrange("p (c t) -> p c t", c=8)
        xav = xa1_s.rearrange("p (c t) -> p c t", c=8)
        hv = h1_s.rearrange("p (c t) -> p c t", c=8)
        chunks = [(0, xe2, 128 + ci * 512, 512, ci * 512) for ci in range(5)] + [(1, ctx2, 128, 256, NE)]
        def loads(ci):
            (j, src, r0, n, t0) = chunks[ci]
            s = ci % 2
            P.dma('sp', ao[s][:, 4:8, :n], aov[:, 4:8, t0:t0 + n], writes=[b_ao[s]])
            P.dma('sp', cvt[s][:, :, :n], cvv[:, :, t0:t0 + n], writes=[b_cvt[s]])
            for ti in range(n // 128):
                P.dma('sp', xt[ti][:], src[r0 + ti * 128:r0 + (ti + 1) * 128, :], writes=[b_xt[ti]])

        def conv_ln(ci):
            (j, src, r0, n, t0) = chunks[ci]
            s = ci % 2
            b_xc2 = b_xc
            ln_stats([cvt[s][:, c, :n] for c in range(4)], n, 512, st2, b_st2, ps1, ps2, b_ps1, b_ps2, [b_cvt[s]])
            TT('dve', xc2[:, :, :n], cvt[s][:, :, :n], bc_mid(st2['mean'][:, :n], 4), ALU.subtract, [b_cvt[s], b_st2['m']], [b_xc2])
            TT('dve', xc2[:, :, :n], xc2[:, :, :n], bc_mid(st2['rstd'][:, :n], 4), ALU.mult, [b_xc2, b_st2['r']], [b_xc2])
            for c in range(4):
                ACT(ao[s][:, c, :n], xc2[:, c, :n], AF.Silu, [b_xc2, b_msc], [b_ao[s]], scale=M("convgb", c), bias=M("convgb", 4 + c))

        def trans(ci):
            (j, src, r0, n, t0) = chunks[ci]
            s = ci % 2
            for ti in range(n // 128):
                for k in range(8):
                    TR(ptr[:, k * 128:(k + 1) * 128], xt[ti][:, k * 128:(k + 1) * 128], identf[:], [b_xt[ti], b_const], [b_ptr])
                ACT(xa[s][:, :, ti * 128:(ti + 1) * 128], ptr[:].rearrange("p (k t) -> p k t", k=8), AF.Copy, [b_ptr], [b_xa[s]], scale=ALPHA)

        loads(0); trans(0); conv_ln(0); loads(1); conv_ln(1)
        for ci, (j, src, r0, n, t0) in enumerate(chunks):
            s = ci % 2
            for m in range(8):
                sp_ = m % 2
                for k in range(8):
                    MM(py[sp_][:, :n], wo[:, k, m * 128:(m + 1) * 128], ao[s][:, k, :n], k == 0, k == 7, [b_w, b_ao[s]], [b_py[sp_]])
                STT(vt[s][:, m, :n], py[sp_][:, :n], MR(0, j, "gm", m), xa[s][:, m, :n], ALU.mult, ALU.add,
                    [b_py[sp_], b_msc, b_xa[s]], [b_vt[s]])
            ln_stats([vt[s][:, m, :n] for m in range(8)], n, 1024, st, b_st, ps1, ps2, b_ps1, b_ps2, [b_vt[s]])
            if ci + 1 < len(chunks):
                trans(ci + 1)
            if ci + 2 < len(chunks):
                loads(ci + 2)
            resid_ln_tail(vt[s], b_vt[s], n, st, b_st, xc, b_xc, oxa[s], oh[s], b_oxa[s], b_oh[s],
                          (f"A1s0{j}", f"A1b0{j}", f"H1s0{j}", f"H1b0{j}"))
            P.dma('sp', xav[:, :, t0:t0 + n], oxa[s][:, :, :n], reads=[b_oxa[s]])
            P.dma('sp', hv[:, :, t0:t0 + n], oh[s][:, :, :n], reads=[b_oh[s]])
            if ci + 2 < len(chunks):
                conv_ln(ci + 2)

    def phaseE2a(es, post_init=None):
        hb = sb(es, "hb", [128, 8, NT0], BF16)
        wgu = [sb(es, f"wgu{s}", [128, 2, 8, 512], BF16) for s in range(2)]
        sg = [sb(es, f"sg{s}", [128, 512]) for s in range(2)]
        gat = [sb(es, f"gat{s}", [128, 4, 512], BF16) for s in range(3)]
        pg = [psum(es, f"pg{s}", [128, 512]) for s in range(3)]; pu = [psum(es, f"pu{s}", [128, 512]) for s in range(3)]
        b_wgu = P.bufs(2); b_sg = P.bufs(2); b_gat = P.bufs(3); b_pg = P.bufs(3); b_pu = P.bufs(3)
        shared['e2'] = (pg, pu, b_pg, b_pu)
        chunks = [(ci * 512, 512) for ci in range(5)] + [(NE, 256)]
        b_hb = P.bufs(len(chunks))
        hv = h1_s.rearrange("p (c t) -> p c t", c=8)
        gav = ga_s.rearrange("p (c t) -> p c t", c=22)
        for ci, (t0, n) in enumerate(chunks):
            P.dma('sp', hb[:, :, t0:t0 + n], hv[:, :, t0:t0 + n], writes=[b_hb[ci]])
        groups = [(0, 4), (4, 4), (8, 4), (12, 4), (16, 4), (20, 2)]
        isg = 0; iq = 0
        for gi, (c0, ncg) in enumerate(groups):
            s = gi % 2
            P.dma_group('pool', [(wgu[s][:, 0, :, :ncg * 128], wg0.rearrange("(k p) c -> p k c", p=128)[:, :, c0 * 128:(c0 + ncg) * 128]),
                                 (wgu[s][:, 1, :, :ncg * 128], wu0.rearrange("(k p) c -> p k c", p=128)[:, :, c0 * 128:(c0 + ncg) * 128])],
                        writes=[b_wgu[s]])
            if gi == 1 and post_init:
                post_init()
            for ci, (t0, n) in enumerate(chunks):
                q = iq % 3; iq += 1
                for cc in range(ncg):
                    sp_ = isg % 3; ss_ = isg % 2; isg += 1
                    for k in range(8):
                        MM(pg[sp_][:, :n], wgu[s][:, 0, k, cc * 128:(cc + 1) * 128], hb[:, k, t0:t0 + n], k == 0, k == 7, [b_wgu[s], b_hb[ci]], [b_pg[sp_]])
                    for k in range(8):
                        MM(pu[sp_][:, :n], wgu[s][:, 1, k, cc * 128:(cc + 1) * 128], hb[:, k, t0:t0 + n], k == 0, k == 7, [b_wgu[s], b_hb[ci]], [b_pu[sp_]])
                    ACT(sg[ss_][:, :n], pg[sp_][:, :n], AF.Silu, [b_pg[sp_]], [b_sg[ss_]])
                    TT('dve', gat[q][:, cc, :n], pu[sp_][:, :n], sg[ss_][:, :n], ALU.mult, [b_pu[sp_], b_sg[ss_]], [b_gat[q]])
                P.dma('sp', gav[:, c0:c0 + ncg, t0:t0 + n], gat[q][:, :ncg, :n], reads=[b_gat[q]])

    def phaseE2b(es, pre=None):
        wd = pre['wd'][0] if pre else sb(es, "wd", [128, 22, D], BF16)
        gab = [sb(es, f"gab{s}", [128, 22, 512], BF16) for s in range(2)]
        xab = [sb(es, f"xab{s}", [128, 8, 512]) for s in range(2)]
        oh = [sb(es, f"oh{s}", [128, 8, 512], BF16) for s in range(2)]
        xc = sb(es, "xcb", [128, 8, 512])
        st, b_st = mk_st(es, "f", LN_EPS)
        py = [psum(es, f"py{s}", [128, 512]) for s in range(3)]
        ps1 = psum(es, "ps1", [128, 512]); ps2 = psum(es, "ps2", [128, 512])
        b_gab = P.bufs(2); b_xab = P.bufs(2); b_oh = P.bufs(2); b_py = P.bufs(3); b_xc = P.buf()
        b_ps1, b_ps2 = P.bufs(2)
        if pre:
            b_wd = pre['wd'][1]
        else:
            b_wd = P.buf()
            P.dma_group('pool', [(wd[:, 0:11, :], wd0.rearrange("(c p) m -> p c m", p=128)[:, 0:11, :]),
                                 (wd[:, 11:22, :], wd0.rearrange("(c p) m -> p c m", p=128)[:, 11:22, :])], writes=[b_wd])
        gav = ga_s.rearrange("p (c t) -> p c t", c=22)
        xav = xa1_s.rearrange("p (c t) -> p c t", c=8)
        hv2 = h2_s.rearrange("p (c t) -> p c t", c=8)
        xav2 = xa2_s.rearrange("p (c t) -> p c t", c=8)
        chunks = [(ci * 512, 512) for ci in range(5)] + [(NE, 256)]
        NCH = len(chunks)
        cpy = {'i': 0}

        def loads(ci):
            t0, n = chunks[ci]; s = ci % 2
            P.dma('sp', gab[s][:, :, :n], gav[:, :, t0:t0 + n], writes=[b_gab[s]])
            P.dma('sp', xab[s][:, :, :n], xav[:, :, t0:t0 + n], writes=[b_xab[s]])

        def mm_stats(ci):
            t0, n = chunks[ci]; s = ci % 2
            j = 1 if t0 >= NE else 0
            for m in range(8):
                sp_ = cpy['i'] % 3; cpy['i'] += 1
                for c in range(22):
                    MM(py[sp_][:, :n], wd[:, c, m * 128:(m + 1) * 128], gab[s][:, c, :n], c == 0, c == 21, [b_wd, b_gab[s]], [b_py[sp_]])
                STT(xab[s][:, m, :n], py[sp_][:, :n], MR(0, j, "gf", m), xab[s][:, m, :n], ALU.mult, ALU.add,
                    [b_py[sp_], b_msc, b_xab[s]], [b_xab[s]])
            ln_stats([xab[s][:, m, :n] for m in range(8)], n, 1024, st, b_st, ps1, ps2, b_ps1, b_ps2, [b_xab[s]])

        def tail(ci):
            t0, n = chunks[ci]; s = ci % 2
            j = 1 if t0 >= NE else 0
            resid_ln_tail(xab[s], b_xab[s], n, st, b_st, xc, b_xc, xab[s], oh[s], b_xab[s], b_oh[s],
                          (f"A2s0{j}", f"A2b0{j}", f"H2s0{j}", f"H2b0{j}"))
            P.dma('sp', xav2[:, :, t0:t0 + n], xab[s][:, :, :n], reads=[b_xab[s]])
            P.dma('sp', hv2[:, :, t0:t0 + n], oh[s][:, :, :n], reads=[b_oh[s]])

        loads(0); loads(1)
        mm_stats(0)
        for ci in range(NCH):
            tail(ci)
            if ci + 1 < NCH:
                mm_stats(ci + 1)
            if ci + 2 < NCH:
                loads(ci + 2)

    def phaseF(es, post_init=None):
        NB = 4; L = 3
        hb = sb(es, "hb", [128, 8, NT0], BF16)
        wq = [sb(es, f"wq{s}", [128, 3, 8, 128], BF16) for s in range(2)]
        qT = [sb(es, f"qT{s}", [128, NOWN], BF16) for s in range(2)]
        kTz = [[sb(es, f"kT{s}{h}", [128, NT0], BF16) for h in range(2)] for s in range(2)]
        va = [sb(es, f"va{s}", [128, 22, 256], BF16) for s in range(2)]
        tb = sb(es, "tb", [128, 2, 14 * 64])
        tI = [sb(es, f"tI{s}", [128, 2, 14 * 64]) for s in range(2)]
        tA = [sb(es, f"tA{s}", [128, 2, 14 * 64]) for s in range(2)]
        msk = sb(es, "msk", [128, 2, 14 * 64])
        vrow = sb(es, "vrow", [128, 48])
        ex = [sb(es, f"ex{s}", [128, 512]) for s in range(NB)]
        pT = [sb(es, f"pT{s}", [128, 512], BF16) for s in range(NB)]
        rs = [sb(es, f"rs{s}", [128, 256]) for s in range(2)]
        ob = [sb(es, f"ob{s}", [128, NOWN], BF16) for s in range(2)]
        pq = [psum(es, f"pq{s}", [128, 512]) for s in range(2)]
        pss = [psum(es, f"pss{s}", [128, 512]) for s in range(NB)]
        po = [psum(es, f"po{s}", [128, 512]) for s in range(2)]
        b_wq = P.bufs(2); b_ex = P.bufs(NB); b_pT = P.bufs(NB); b_rs = P.bufs(2); b_pq = P.bufs(2); b_pss = P.bufs(NB); b_po = P.bufs(2)
        b_qT = P.bufs(2); b_kT = P.bufs(2); b_va = P.bufs(2); b_tab = P.bufs(2); b_ob = P.bufs(2)
        b_hb, b_tb, b_msk = P.bufs(3)
        P.dma('sp', hb[:], h2_s.rearrange("p (c t) -> p c t", c=8), writes=[b_hb])
        P.dma('sp', msk[:], namask.rearrange("p (a f) -> p a f", a=2), writes=[b_msk])
        P.dma('sp', vrow[:], navrow, writes=[b_msk])
        for s in range(2):
            MEMSET('pool', va[s][:], 1.0, [b_va[s]])
            MEMSET('dve', kTz[s][0][64:128, :], 0.0, [b_kT[s]])
            MEMSET('dve', kTz[s][1][0:64, :], 0.0, [b_kT[s]])
        o1v = o1_s.rearrange("p (c t) -> p c t", c=8)
        cnt = {'pq': 0}

        def prologue(pr):
            s = pr % 2
            P.dma_group('pool', [(wq[s][:, a, :, :], w_qkv.rearrange("(k p) c -> p k c", p=128)[:, :, a * D + pr * 128:a * D + (pr + 1) * 128])
                                 for a in range(3)], writes=[b_wq[s]])
            P.dma_group('sp', [(tb[:, hh, :], nabias[pr * 2 + hh]) for hh in range(2)], writes=[b_tb])
            ACT(tb[:], tb[:], AF.Exp, [b_tb], [b_tb])
            TT('pool', tI[s][:], tb[:], bc_mid(msk[:, 0, :], 2), ALU.mult, [b_tb, b_msk], [b_tab[s]])
            TT('pool', tA[s][:], tb[:], bc_mid(msk[:, 1, :], 2), ALU.mult, [b_tb, b_msk], [b_tab[s]])
            for ci in range(4):
                sp_ = cnt['pq'] % 2; cnt['pq'] += 1
                for k in range(8):
                    MM(pq[sp_][:, :], wq[s][:, 0, k, :], hb[:, k, 256 + ci * 512:256 + (ci + 1) * 512], k == 0, k == 7, [b_wq[s], b_hb], [b_pq[sp_]])
                ACT(qT[s][:, ci * 512:(ci + 1) * 512], pq[sp_][:, :], AF.Copy, [b_pq[sp_]], [b_qT[s]], scale=0.125)
            for ci in range(6):
                n = 512 if ci < 5 else 256
                sp_ = cnt['pq'] % 2; cnt['pq'] += 1
                for k in range(8):
                    MM(pq[sp_][:, :n], wq[s][:, 1, k, :], hb[:, k, ci * 512:ci * 512 + n], k == 0, k == 7, [b_wq[s], b_hb], [b_pq[sp_]])
                CP('dve', kTz[s][0][0:64, ci * 512:ci * 512 + n], pq[sp_][0:64, :n], [b_pq[sp_]], [b_kT[s]])
                CP('act', kTz[s][1][64:128, ci * 512:ci * 512 + n], pq[sp_][64:128, :n], [b_pq[sp_]], [b_kT[s]])
            for t4 in range(0, 22, 4):
                nt = min(4, 22 - t4)
                sp_ = cnt['pq'] % 2; cnt['pq'] += 1
                for ti in range(nt):
                    t = t4 + ti
                    for k in range(8):
                        MM(pq[sp_][:, ti * 128:(ti + 1) * 128], hb[:, k, t * 128:(t + 1) * 128], wq[s][:, 2, k, :], k == 0, k == 7, [b_hb, b_wq[s]], [b_pq[sp_]])
                pv4 = pq[sp_][:, :nt * 128].rearrange("p (t h d) -> p t h d", h=2, d=64)
                CP('act', va[s][:, t4:t4 + nt, 0:64], pv4[:, :, 0, :], [b_pq[sp_]], [b_va[s]])
                CP('act', va[s][:, t4:t4 + nt, 192:256], pv4[:, :, 1, :], [b_pq[sp_]], [b_va[s]])

        items = [(pr, g, j) for pr in range(8) for g in range(8) for j in range(8)]

        def key_of(g, j):
            return (2 * g + j, j) if j < 6 else (20 + (j - 6), None)

        def qk_exp(i):
            pr, g, j = items[i]
            if g == 0 and j == 0:
                prologue(pr)
                if pr == 1 and post_init:
                    post_init()
            s = pr % 2; s3 = i % NB; q0 = g * 256
            kt, ch = key_of(g, j)
            special = g in (0, 7)
            for hh in range(2):
                MM(pss[s3][:, hh * 256:(hh + 1) * 256], kTz[s][hh][:, kt * 128:(kt + 1) * 128], qT[s][:, q0:q0 + 256], True, True,
                   [b_kT[s], b_qT[s]], [b_pss[s3]])
            if ch is None:
                ACT(pT[s3][:, :], pss[s3][:, :], AF.Exp, [b_pss[s3]], [b_pT[s3]])
            else:
                ACT(ex[s3][:, :], pss[s3][:, :], AF.Exp, [b_pss[s3]], [b_ex[s3]])
                sl = 10 - 2 * ch
                exv = ex[s3][:, :].rearrange("p (h q) -> p h q", h=2)
                pTv = pT[s3][:, :].rearrange("p (h q) -> p h q", h=2)
                if special:
                    TT('dve', exv, exv, tA[s][:, :, sl * 64:(sl + 4) * 64], ALU.mult, [b_ex[s3], b_tab[s]], [b_ex[s3]])
                    vi = (0 if g == 0 else 1) * 24 + ch * 4
                    va_ = vrow[:, vi:vi + 4]
                    vb = bass.AP(tensor=va_.tensor, offset=va_.offset, ap=[list(va_.ap[0]), [0, 2], [1, 4], [0, 64]])
                    TT('dve', pT[s3][:, :].rearrange("p (h i q) -> p h i q", h=2, i=4), ex[s3][:, :].rearrange("p (h i q) -> p h i q", h=2, i=4),
                       vb, ALU.mult, [b_ex[s3], b_msk], [b_pT[s3]])
                else:
                    TT('dve', pTv, exv, tI[s][:, :, sl * 64:(sl + 4) * 64], ALU.mult, [b_ex[s3], b_tab[s]], [b_pT[s3]])

        def pv(i):
            pr, g, j = items[i]
            s = pr % 2; s3 = i % NB; q0 = g * 256
            kt, ch = key_of(g, j)
            for hh in range(2):
                lo = 0 if hh == 0 else 128
                MM(po[hh][:, :256], va[s][:, kt, lo:lo + 128], pT[s3][:, hh * 256:(hh + 1) * 256], j == 0, j == 7, [b_va[s], b_pT[s3]], [b_po[hh]])
            if j == 7:
                for hh in range(2):
                    ob_, sm_ = (0, 64) if hh == 0 else (64, 0)
                    ACT(rs[hh][ob_:ob_ + 64, :], po[hh][sm_:sm_ + 64, :256], AF.Ln, [b_po[hh]], [b_rs[hh]])
                    ACT(rs[hh][ob_:ob_ + 64, :], rs[hh][ob_:ob_ + 64, :], AF.Exp, [b_rs[hh]], [b_rs[hh]], scale=-1.0)
                    TT('dve', ob[s][ob_:ob_ + 64, q0:q0 + 256], po[hh][ob_:ob_ + 64, :256], rs[hh][ob_:ob_ + 64, :], ALU.mult,
                       [b_po[hh], b_rs[hh]], [b_ob[s]])
                if g == 7:
                    P.dma('sp', o1v[:, pr, :], ob[s][:], reads=[b_ob[s]])

        for i in range(len(items) + L):
            if i < len(items):
                qk_exp(i)
            if i >= L:
                pv(i - L)

    def phaseG(es, pre=None):
        wo = pre['wo'][0] if pre else sb(es, "wo", [128, 8, D], BF16)
        wr = pre['wr'][0] if pre else sb(es, "wr", [128, 8, 8], BF16)
        ao = [sb(es, f"ao{s}", [128, 8, 512], BF16) for s in range(2)]
        xa = [sb(es, f"xa{s}", [128, 8, 512]) for s in range(2)]
        vt = [sb(es, f"vt{s}", [128, 8, 512]) for s in range(2)]
        oxa = [sb(es, f"oxa{s}", [128, 8, 512]) for s in range(2)]
        oh = [sb(es, f"oh{s}", [128, 8, 512], BF16) for s in range(2)]
        xc = sb(es, "xcb", [128, 8, 512])
        xtm = [sb(es, f"xtm{s}", [128, D]) for s in range(2)]
        lg = sb(es, "lg", [128, 8]); mx = sb(es, "mx", [128, 8]); eq = sb(es, "eq", [128, 8]); wts = sb(es, "wts", [128, 4])
        st, b_st = mk_st(es, "g", LN_EPS)
        ptr = psum(es, "ptr", [128, 1024]); py = [psum(es, f"py{s}", [128, 512]) for s in range(2)]
        ps1 = psum(es, "ps1", [128, 512]); ps2 = psum(es, "ps2", [128, 512]); plg = psum(es, "plg", [128, 512])
        b_ao = P.bufs(2); b_xa = P.bufs(2); b_vt = P.bufs(2); b_oxa = P.bufs(2); b_oh = P.bufs(2); b_xc = P.buf(); b_py = P.bufs(2); b_xtm = P.bufs(2)
        b_w, b_ptr, b_ps1, b_ps2, b_plg, b_lg = P.bufs(6)
        if pre:
            b_w = pre['wo'][1]
        else:
            P.dma_group('pool', [(wo[:], w_out1.rearrange("(k p) c -> p k c", p=128)),
                                 (wr[:], w_router.rearrange("(k p) c -> p k c", p=128))], writes=[b_w])
        o1v = o1_s.rearrange("p (c t) -> p c t", c=8)
        xav = xa2_s.rearrange("p (c t) -> p c t", c=8)
        hv = h3_s.rearrange("p (c t) -> p c t", c=8)
        tix = 0

        def prep(ci):
            s = ci % 2; n = 512; t0 = ci * 512
            P.dma('sp', ao[s][:], o1v[:, :, t0:t0 + n], writes=[b_ao[s]])
            P.dma('sp', xa[s][:], xav[:, :, 256 + t0:256 + t0 + n], writes=[b_xa[s]])

        tixc = {'i': 0}

        def mm_stats(ci):
            s = ci % 2; n = 512
            for m in range(8):
                sp_ = m % 2
                for k in range(8):
                    MM(py[sp_][:, :n], wo[:, k, m * 128:(m + 1) * 128], ao[s][:, k, :n], k == 0, k == 7, [b_w, b_ao[s]], [b_py[sp_]])
                STT(vt[s][:, m, :n], py[sp_][:, :n], MR(1, 0, "gm", m), xa[s][:, m, :n], ALU.mult, ALU.add,
                    [b_py[sp_], b_msc, b_xa[s]], [b_vt[s]])
            ln_stats([vt[s][:, m, :n] for m in range(8)], n, 1024, st, b_st, ps1, ps2, b_ps1, b_ps2, [b_vt[s]])

        def tail(ci):
            s = ci % 2; n = 512; t0 = ci * 512
            resid_ln_tail(vt[s], b_vt[s], n, st, b_st, xc, b_xc, oxa[s], oh[s], b_oxa[s], b_oh[s],
                          ("A1s10", "A1b10", "H1s10", "H1b10"))
            P.dma('sp', hv[:, :, t0:t0 + n], oh[s][:], reads=[b_oh[s]])

        def post(ci):
            s = ci % 2
            for ti in range(4):
                s2 = tixc['i'] % 2; tixc['i'] += 1
                tile_i = ci * 4 + ti
                for k in range(8):
                    TR(ptr[:, k * 128:(k + 1) * 128], oxa[s][:, k, ti * 128:(ti + 1) * 128], identf[:], [b_oxa[s], b_const], [b_ptr])
                CP('act', xtm[s2][:], ptr[:], [b_ptr], [b_xtm[s2]])
                P.dma('sp', xa3_s[tile_i * 128:(tile_i + 1) * 128, :], xtm[s2][:], reads=[b_xtm[s2]])
                for k in range(8):
                    MM(plg[:, 0:8], oh[s][:, k, ti * 128:(ti + 1) * 128], wr[:, k, :], k == 0, k == 7, [b_oh[s], b_w], [b_plg])
                CP('dve', lg[:], plg[:, 0:8], [b_plg], [b_lg])
                P.op('dve', lambda e: e.max(out=mx[:], in_=lg[:]), [b_lg], [b_lg])
                TT('dve', wts[:, 0:1], mx[:, 0:1], mx[:, 1:2], ALU.subtract, [b_lg], [b_lg])
                TT('dve', wts[:, 1:2], mx[:, 1:2], mx[:, 0:1], ALU.subtract, [b_lg], [b_lg])
                ACT(wts[:, 2:4], wts[:, 0:2], AF.Sigmoid, [b_lg], [b_lg])
                gt = gates[:, tile_i * 8:(tile_i + 1) * 8]
                TS('dve', eq[:], lg[:], mx[:, 0:1], wts[:, 2:3], ALU.is_equal, ALU.mult, [b_lg], [b_lg])
                TS('dve', gt, lg[:], mx[:, 1:2], wts[:, 3:4], ALU.is_equal, ALU.mult, [b_lg], [b_gates])
                TT('dve', gt, gt, eq[:], ALU.add, [b_lg, b_gates], [b_gates])

        prep(0); prep(1)
        mm_stats(0)
        for ci in range(4):
            tail(ci)
            if ci + 1 < 4:
                mm_stats(ci + 1)
            post(ci)
            if ci + 2 < 4:
                prep(ci + 2)

    def phaseH(es):
        hb = sb(es, "hb", [128, 8, NOWN], BF16)
        yacc = sb(es, "yacc", [128, 16, D])
        wgu = [sb(es, f"wgu{s}", [128, 2, 8, 512], BF16) for s in range(2)]
        wdn = [sb(es, f"wdn{s}", [128, 4, D], BF16) for s in range(2)]
        sg = [sb(es, f"sg{s}", [128, 512]) for s in range(2)]
        ga = [sb(es, f"ga{s}", [128, 4, 512], BF16) for s in range(2)]
        pg = [psum(es, f"pg{s}", [128, 512]) for s in range(2)]; pu = [psum(es, f"pu{s}", [128, 512]) for s in range(2)]
        py = [psum(es, f"py{s}", [128, 512]) for s in range(4)]
        b_wgu = P.bufs(2); b_wdn = P.bufs(2); b_sg = P.bufs(2); b_ga = P.bufs(2); b_pg = P.bufs(2); b_pu = P.bufs(2); b_py = P.bufs(4)
        b_hb4 = P.bufs(4)
        b_y = P.bufs(16)
        h3v = h3_s.rearrange("p (c t) -> p c t", c=8)
        for tcq in range(4):
            P.dma('sp', hb[:, :, tcq * 512:(tcq + 1) * 512], h3v[:, :, tcq * 512:(tcq + 1) * 512], writes=[b_hb4[tcq]])
        cnts = {'isg': 0, 'ipy': 0}
        steps = [(e, cg, tc) for e in range(NEXP) for cg in range(7) for tc in range(4)]

        def gate_up(i):
            e, cg, tc = steps[i]
            gi = e * 7 + cg
            s = gi % 2
            c0 = cg * 4
            if tc == 0:
                P.dma_group('pool', [(wgu[s][:, 0, :, :], mwg[e].rearrange("(k p) c -> p k c", p=128)[:, :, c0 * 128:(c0 + 4) * 128]),
                                     (wgu[s][:, 1, :, :], mwu[e].rearrange("(k p) c -> p k c", p=128)[:, :, c0 * 128:(c0 + 4) * 128])],
                            writes=[b_wgu[s]])
                P.dma('pool', wdn[s][:], mwd[e].rearrange("(c p) m -> p c m", p=128)[:, c0:c0 + 4, :], writes=[b_wdn[s]])
            o = tc * 512
            sga = i % 2
            for cc in range(4):
                sp_ = cnts['isg'] % 2; cnts['isg'] += 1
                for k in range(8):
                    MM(pg[sp_][:, :], wgu[s][:, 0, k, cc * 128:(cc + 1) * 128], hb[:, k, o:o + 512], k == 0, k == 7, [b_wgu[s], b_hb4[tc]], [b_pg[sp_]])
                for k in range(8):
                    MM(pu[sp_][:, :], wgu[s][:, 1, k, cc * 128:(cc + 1) * 128], hb[:, k, o:o + 512], k == 0, k == 7, [b_wgu[s], b_hb4[tc]], [b_pu[sp_]])
                ACT(sg[sp_][:, :], pg[sp_][:, :], AF.Silu, [b_pg[sp_]], [b_sg[sp_]])
                TT('dve', ga[sga][:, cc, :], pu[sp_][:, :], sg[sp_][:, :], ALU.mult, [b_pu[sp_], b_sg[sp_]], [b_ga[sga]])

        def down(i):
            e, cg, tc = steps[i]
            gi = e * 7 + cg
            s = gi % 2
            sga = i % 2
            first = (e == 0 and cg == 0)
            for ti in range(4):
                tile_i = tc * 4 + ti
                for hf in range(2):
                    sy = cnts['ipy'] % 4; cnts['ipy'] += 1
                    for cc in range(4):
                        MM(py[sy][:, :], ga[sga][:, cc, ti * 128:(ti + 1) * 128], wdn[s][:, cc, hf * 512:(hf + 1) * 512], cc == 0, cc == 3,
                           [b_ga[sga], b_wdn[s]], [b_py[sy]])
                    ya = yacc[:, tile_i, hf * 512:(hf + 1) * 512]
                    gsc = gates[:, tile_i * 8 + e:tile_i * 8 + e + 1]
                    if first:
                        TS('dve', ya, py[sy][:, :], gsc, None, ALU.mult, None, [b_py[sy], b_gates], [b_y[tile_i]])
                    else:
                        STT(ya, py[sy][:, :], gsc, ya, ALU.mult, ALU.add, [b_py[sy], b_gates, b_y[tile_i]], [b_y[tile_i]])

        for i in range(len(steps) + 1):
            if i < len(steps):
                gate_up(i)
            if i >= 1:
                down(i - 1)
        for t in range(16):
            P.dma('sp', y_s[t * 128:(t + 1) * 128, :], yacc[:, t, :], reads=[b_y[t]])

    def phaseI(es):
        lnr = sb(es, "lnr", [128, 2 * D])
        gfr = sb(es, "gfr", [128, D])
        scb = sb(es, "scb", [128, 8, 2], BF16)
        wmr = sb(es, "wmr", [128, 8, D], BF16)
        row = sb(es, "row", [1, D]); brow = sb(es, "brow", [1, D])
        xr = [sb(es, f"xr{s}", [128, D]) for s in range(3)]
        yr = [sb(es, f"yr{s}", [128, D]) for s in range(3)]
        vv = [sb(es, f"vv{s}", [128, D]) for s in range(3)]
        stt3 = [sb(es, f"stt{q}", [128, 2, 6]) for q in range(3)]; ag3 = [sb(es, f"ag{q}", [128, 2]) for q in range(3)]
        rstd3 = [sb(es, f"rstd{q}", [128, 1]) for q in range(3)]; nmr3 = [sb(es, f"nmr{q}", [128, 1]) for q in range(3)]
        epsb = sb(es, "epsb", [128, 1]); b_st3 = P.bufs(3)
        csb = sb(es, "csb", [128, 16])
        pg = [psum(es, "pgi", [128, 512])]; pu = [psum(es, "pui", [128, 512])]
        b_pg = P.bufs(1); b_pu = P.bufs(1)
        b_xr = P.bufs(3); b_vv = P.bufs(3); b_yr = P.bufs(3)
        b_lnr, b_gfr, b_row, b_st = P.bufs(4)
        P.dma('sp', lnr[:], lnrow, writes=[b_lnr])
        MEMSET('dve', epsb[:], LN_EPS, [b_st])
        P.dma('sp', csb[:], cT, writes=[b_row])
        ACT(scb[:].rearrange("p k j -> p (k j)"), csb[:], AF.Silu, [b_row], [b_row])
        P.dma('pool', wmr[:], w_mod[1].rearrange("(k p) c -> p k c", p=128)[:, :, 5 * D:6 * D], writes=[b_row])
        P.dma('sp', brow[:], bmodrow, writes=[b_row])
        for hf in range(2):
            for k in range(8):
                MM(pg[0][0:2, :], scb[:, k, :], wmr[:, k, hf * 512:(hf + 1) * 512], k == 0, k == 7, [b_row], [b_pg[0]])
            TT('dve', row[0:1, hf * 512:(hf + 1) * 512], pg[0][0:1, :], brow[0:1, hf * 512:(hf + 1) * 512], ALU.add, [b_pg[0], b_row], [b_row])
        for hf in range(2):
            MM(pu[0][:, :], onesf[0:1, :], row[0:1, hf * 512:(hf + 1) * 512], True, True, [b_row, b_const], [b_pu[0]])
            CP('dve', gfr[:, hf * 512:(hf + 1) * 512], pu[0][:, :], [b_pu[0]], [b_gfr])
        def prep(t):
            s = t % 3
            P.dma('sp', xr[s][:], xa3_s[t * 128:(t + 1) * 128, :], writes=[b_xr[s]])
            P.dma('sp', yr[s][:], y_s[t * 128:(t + 1) * 128, :], writes=[b_yr[s]])

        prep(0); prep(1)
        for t in range(16):
            s = t % 3
            if t + 2 < 16:
                prep(t + 2)
            TT('dve', vv[s][:], yr[s][:], gfr[:], ALU.mult, [b_yr[s], b_gfr], [b_vv[s]])
            TT('dve', vv[s][:], vv[s][:], xr[s][:], ALU.add, [b_vv[s], b_xr[s]], [b_vv[s]])
            stt, ag, rstd, nmr, b_sq = stt3[s], ag3[s], rstd3[s], nmr3[s], b_st3[s]
            for hf in range(2):
                P.op('dve', lambda e, o_=stt[:, hf, :], i_=vv[s][:, hf * 512:(hf + 1) * 512]: e.bn_stats(out=o_, in_=i_), [b_vv[s]], [b_sq])
            P.op('dve', lambda e, o_=ag[:], i_=stt[:].rearrange("p a b -> p (a b)"): e.bn_aggr(out=o_, in_=i_), [b_sq], [b_sq])
            ACT(rstd[:], ag[:, 1:2], AF.Sqrt, [b_sq, b_st], [b_sq], bias=epsb[:, 0:1])
            RECIP(rstd[:], rstd[:], [b_sq], [b_sq])
            TS('dve', vv[s][:], vv[s][:], ag[:, 0:1], rstd[:, 0:1], ALU.subtract, ALU.mult, [b_vv[s], b_sq], [b_vv[s]])
            TT('dve', vv[s][:], vv[s][:], lnr[:, 0:D], ALU.mult, [b_vv[s], b_lnr], [b_vv[s]])
            TT('dve', vv[s][:], vv[s][:], lnr[:, D:2 * D], ALU.add, [b_vv[s], b_lnr], [b_vv[s]])
            P.dma('sp', out_d[t * 128:(t + 1) * 128, :], vv[s][:], reads=[b_vv[s]])

    def run(fn, **kw):
        with contextlib.ExitStack() as es:
            fn(es, **kw)
            P.flush()

    ES = contextlib.ExitStack
    run(phase0)
    with ES() as o:
        wA = sb(o, "wA_pre", [128, 8, 1536], BF16); b_wA = P.buf()
        run(phaseA, post_init=lambda: P.dma('pool', wA[:], w_in.rearrange("(k p) c -> p k c", p=128)[:, :, 0:1536], writes=[b_wA]))
        run(phaseB, pre={'wA': (wA, b_wA)})
    with ES() as o:
        wo0 = sb(o, "wo0_pre", [128, 8, D], BF16); b_wo0 = P.buf()
        run(phaseD, post_init=lambda: P.dma('pool', wo0[:], w_out0.rearrange("(k p) c -> p k c", p=128), writes=[b_wo0]))
        run(phaseE1, pre={'wo': (wo0, b_wo0)})
    with ES() as o:
        wdp = sb(o, "wd_pre", [128, 22, D], BF16); b_wdp = P.buf()
        run(phaseE2a, post_init=lambda: P.dma_group('pool', [(wdp[:, 0:11, :], wd0.rearrange("(c p) m -> p c m", p=128)[:, 0:11, :]),
                                                              (wdp[:, 11:22, :], wd0.rearrange("(c p) m -> p c m", p=128)[:, 11:22, :])], writes=[b_wdp]))
        run(phaseE2b, pre={'wd': (wdp, b_wdp)})
    with ES() as o:
        wo1 = sb(o, "wo1_pre", [128, 8, D], BF16); wr1 = sb(o, "wr1_pre", [128, 8, 8], BF16); b_wo1 = P.buf()
        run(phaseF, post_init=lambda: P.dma_group('pool', [(wo1[:], w_out1.rearrange("(k p) c -> p k c", p=128)),
                                                            (wr1[:], w_router.rearrange("(k p) c -> p k c", p=128))], writes=[b_wo1]))
        run(phaseG, pre={'wo': (wo1, b_wo1), 'wr': (wr1, b_wo1)})
    run(phaseH)
    run(phaseI)
    P.close()
    return nc


def _rope_tables(pos_r, pos_c, scale):
    half = 16
    inv = 10000.0 ** (-np.arange(half, dtype=np.float32) * 2.0 / 32)
    ar = pos_r.astype(np.float32)[:, None] * inv[None, :]
    ac = pos_c.astype(np.float32)[:, None] * inv[None, :]
    cr, sr, cc, sc = np.cos(ar), np.sin(ar), np.cos(ac), np.sin(ac)
    cos = np.concatenate([cr, cr, cc, cc], axis=1)
    sin = np.concatenate([-sr, sr, -sc, sc], axis=1)
    return (np.concatenate([cos, sin], axis=1) * scale).astype(np.float32)


def _core_inputs(b, half, inp):
    x = inp['x'][b]; ctx = inp['ctx'][b]
    own = half * 2048
    d = {}
    idx2 = own - 384 + np.arange(NE2)
    ok2 = (idx2 >= 0) & (idx2 < NALL)
    xe2 = np.zeros((NE2, D), np.float32); xe2[ok2] = x[idx2[ok2]]
    d['xe2'] = xe2
    d['xall'] = np.ascontiguousarray(x)
    c2 = np.zeros((NC2, D), np.float32); c2[128:128 + NC] = ctx
    d['ctx2'] = c2
    mc = np.zeros(NC2, np.float32); mc[128:128 + NC] = 1
    d['me2'] = np.ascontiguousarray(np.broadcast_to(np.concatenate([ok2.astype(np.float32), mc])[None, :], (128, NE2 + NC2)))
    idxe = own - 256 + np.arange(NE)
    oke = (idxe >= 0) & (idxe < NALL)
    pe = np.where(oke, idxe, 0)
    rq = _rope_tables(pe // 64, pe % 64, 0.125)
    ident = np.concatenate([np.ones((NC, 64), np.float32), np.zeros((NC, 64), np.float32)], axis=1)
    d['ropeq'] = np.concatenate([rq, ident * 0.125], axis=0)
    pa = np.arange(NALL)
    d['ropek'] = np.concatenate([ident, _rope_tables(pa // 64, pa % 64, 1.0)], axis=0)
    cT = np.zeros((128, 8, 2), np.float32)
    cT[:, :, 0] = inp['c'][b].reshape(8, 128).T; cT[:, :, 1] = inp['c_ctx'].reshape(8, 128).T
    d['cT'] = cT.reshape(128, 16)
    d['bmodT'] = np.ascontiguousarray(inp['b_mod'].reshape(2, 48, 128).transpose(2, 0, 1).reshape(128, 96))
    ln = np.stack([inp['ln_g'], inp['ln_b']], axis=2)
    d['lnT'] = np.ascontiguousarray(ln.reshape(2, 2, 2, 8, 128).transpose(4, 0, 1, 2, 3).reshape(128, 64))
    d['convwT'] = np.ascontiguousarray(inp['ab_conv_w'][0].reshape(31, 4, 128).transpose(2, 1, 0).reshape(128, 124))
    cgb = np.stack([inp['ab_conv_g'][0], inp['ab_conv_b'][0]], axis=0)
    d['convgbT'] = np.ascontiguousarray(cgb.reshape(2, 4, 128).transpose(2, 0, 1).reshape(128, 8))
    d['qkg'] = np.ascontiguousarray(np.broadcast_to(np.concatenate([inp['ab_q_g'][0], inp['ab_k_g'][0]])[None, :], (128, 128)))
    d['identf'] = np.eye(128, dtype=np.float32)
    for k in ('w_mod',):
        d[k] = inp[k]
    d['ab_w_in'] = inp['ab_w_in'][0]; d['ab_w_out'] = inp['ab_w_out'][0]
    d['ffn_w_gate'] = inp['ffn_w_gate'][0]; d['ffn_w_up'] = inp['ffn_w_up'][0]; d['ffn_w_down'] = inp['ffn_w_down'][0]
    d['na_w_qkv'] = inp['na_w_qkv'][0]; d['na_w_out'] = inp['na_w_out'][0]
    d['moe_w_router'] = inp['moe_w_router'][0]
    d['moe_w_gate'] = inp['moe_w_gate'][0]; d['moe_w_up'] = inp['moe_w_up'][0]; d['moe_w_down'] = inp['moe_w_down'][0]
    rpb = inp['na_rpb'][0]
    kc = np.arange(64)[:, None]; qc = np.arange(64)[None, :]
    dc = np.clip(kc - qc + 15, 0, 30)
    cstart = np.clip(np.arange(64) - 8, 0, 48)[None, :]
    colok = ((kc >= cstart) & (kc < cstart + 16)).astype(np.float32)
    nb = np.zeros((16, 128, 14, 64), np.float32)
    mI = np.zeros((128, 14, 64), np.float32); mA = np.zeros((128, 14, 64), np.float32)
    for hf in range(2):
        for sp in range(14):
            dr = (13 - sp) + hf
            nb[:, hf * 64:(hf + 1) * 64, sp, :] = rpb[:, dr][:, dc]
            mA[hf * 64:(hf + 1) * 64, sp, :] = colok
            if 3 <= dr <= 10:
                mI[hf * 64:(hf + 1) * 64, sp, :] = colok
    d['nabias'] = nb.reshape(16, 128, 14 * 64)
    d['namask'] = np.concatenate([mI.reshape(128, -1), mA.reshape(128, -1)], axis=1)
    vr = np.zeros((128, 2, 6, 4), np.float32)
    for which, g in enumerate((0, 7)):
        for ch in range(6):
            for i in range(4):
                qr = half * 32 + 4 * g + i
                r0 = min(max(qr - 4, 0), 56)
                for hf in range(2):
                    kr = half * 32 + 4 * g - 4 + 2 * ch + hf
                    if r0 <= kr < r0 + 8:
                        vr[hf * 64:(hf + 1) * 64, which, ch, i] = 1
    d['navrow'] = vr.reshape(128, 48)
    lr = np.concatenate([inp['ln_g'][1, 1], inp['ln_b'][1, 1]])
    d['lnrow'] = np.ascontiguousarray(np.broadcast_to(lr[None, :], (128, 2 * D)))
    d['bmodrow'] = np.ascontiguousarray(inp['b_mod'][1, 5 * D:6 * D][None, :])
    return d


_NC_CACHE = {}


def kernel(**inputs):
    inp = {k: np.asarray(v, dtype=np.float32) for k, v in inputs.items()}
    if 'nc' not in _NC_CACHE:
        _NC_CACHE['nc'] = build_program()
    nc = _NC_CACHE['nc']
    in_maps = [_core_inputs(r // 2, r % 2, inp) for r in range(8)]
    res = run_bass_kernel_spmd(nc, in_maps, core_ids=list(range(8)))
    out = np.zeros((4, 4096, D), np.float32)
    for r in range(8):
        b, half = r // 2, r % 2
        out[b, half * 2048:(half + 1) * 2048] = res.results[r]["out"]
    return out
```
